# Optimizing a Trainium2 kernel written in Bass

```python
import jax, jax.numpy as jnp
from jax import lax
import numpy as np

D_MODEL = 1024
BATCH = 8
SEQ = 8192
DEPTH = 2

HEAD_DIM = 64
N_Q_HEADS = 8
N_KV_HEADS = 2
Q_PER_KV = N_Q_HEADS // N_KV_HEADS
ATTN_WIDTH = N_Q_HEADS * HEAD_DIM
KV_WIDTH = N_KV_HEADS * HEAD_DIM
WINDOW = 128
ATTN_BLOCK = WINDOW
ROT_DIM = HEAD_DIM // 4
ROPE_THETA = 500000.0
POOL_WINDOWS = (2, 4, 8, 16)
N_POOL_GROUPS = len(POOL_WINDOWS)
POOL_WIDTH = D_MODEL - ATTN_WIDTH
POOL_GROUP_DIM = POOL_WIDTH // N_POOL_GROUPS
MIX_WIDTH = ATTN_WIDTH + POOL_WIDTH
IN_WIDTH = ATTN_WIDTH + 2 * KV_WIDTH + POOL_WIDTH
N_EXPERTS = 32
TOP_K = 4
D_FF = D_MODEL
SWIGLU_LIMIT = 7.0
SWIGLU_ALPHA = 1.702
MOE_BLOCK = 256
EPS = 1e-6
MAX_POS_OFFSET = 4096

kernel_name = "hybrid_swa_sink_pool_moe_adaln"

F32 = jnp.float32


def rmsnorm(t, g):
    tf = t.astype(F32)
    y = tf * lax.rsqrt(jnp.mean(tf * tf, axis=-1, keepdims=True) + EPS)
    return (y * g.astype(F32)).astype(t.dtype)


def partial_rope(t, cos, sin):
    half = ROT_DIM // 2
    tf = t.astype(F32)
    t1 = tf[..., :half]
    t2 = tf[..., half:ROT_DIM]
    out = jnp.concatenate([t1 * cos - t2 * sin, t2 * cos + t1 * sin, tf[..., ROT_DIM:]], axis=-1)
    return out.astype(t.dtype)


def sliding_window_attention(q, k, v, sinks):
    b, s_len = q.shape[0], q.shape[1]
    T = ATTN_BLOCK
    nb = s_len // T
    qb = q.reshape(b, nb, T, N_KV_HEADS, Q_PER_KV, HEAD_DIM)

    def band(t):
        tb = t.reshape(b, nb, T, N_KV_HEADS, HEAD_DIM)
        prev = jnp.pad(tb[:, :-1], ((0, 0), (1, 0), (0, 0), (0, 0), (0, 0)))
        return jnp.concatenate([prev, tb], axis=2)

    kk, vv = band(k), band(v)
    scores = jnp.einsum('bnqhgd,bnkhd->bnhgqk', qb, kk,
                        preferred_element_type=F32) * (HEAD_DIM ** -0.5)
    qi = jnp.arange(T)[:, None]
    kj = jnp.arange(2 * T)[None, :]
    diff = qi + T - kj
    blk = jnp.arange(nb)[:, None, None]
    allowed = (diff >= 0) & (diff < WINDOW) & ((blk > 0) | (kj >= T))
    scores = jnp.where(allowed[None, :, None, None], scores, -jnp.inf)
    sink = sinks.astype(F32).reshape(1, 1, N_KV_HEADS, Q_PER_KV, 1, 1)
    m = jnp.maximum(jnp.max(scores, axis=-1, keepdims=True), sink)
    p = jnp.exp(scores - m)
    denom = jnp.sum(p, axis=-1, keepdims=True) + jnp.exp(sink - m)
    o = jnp.einsum('bnhgqk,bnkhd->bnhgqd', p.astype(v.dtype), vv,
                   preferred_element_type=F32) / denom
    o = jnp.transpose(o, (0, 1, 4, 2, 3, 5))
    return o.reshape(b, s_len, ATTN_WIDTH).astype(q.dtype)


def multiscale_pool(u, w_pool, b_pool, scale):
    b, s_len = u.shape[0], u.shape[1]
    ug = u.astype(F32).reshape(b, s_len, N_POOL_GROUPS, POOL_GROUP_DIM)
    cs = jnp.cumsum(ug, axis=1)
    count = jnp.arange(1, s_len + 1, dtype=F32)
    outs = []
    for g, w in enumerate(POOL_WINDOWS):
        c_g = cs[:, :, g]
        lag = jnp.pad(c_g, ((0, 0), (w, 0), (0, 0)))[:, :s_len]
        mean = (c_g - lag) / jnp.minimum(count, float(w))[None, :, None]
        outs.append(mean - ug[:, :, g])
    pooled = jnp.stack(outs, axis=2).astype(u.dtype)
    y = jnp.einsum('bsgc,gcd->bsgd', pooled, w_pool) + b_pool
    y = y * scale.reshape(N_POOL_GROUPS, POOL_GROUP_DIM)
    return y.reshape(b, s_len, POOL_WIDTH)


def moe(h, router_w, router_b, w_gu, b_gu, w_down, b_down):
    b, s_len, d = h.shape
    n_tok = b * s_len
    hf = h.reshape(n_tok, d)
    logits = (hf @ router_w).astype(F32) + router_b.astype(F32)
    top_vals, top_idx = lax.top_k(logits, TOP_K)
    gates = jax.nn.softmax(top_vals, axis=-1)
    n_asg = n_tok * TOP_K
    e_flat = top_idx.reshape(n_asg)
    tok_flat = jnp.arange(n_asg, dtype=jnp.int32) // TOP_K
    g_flat = gates.reshape(n_asg)
    order = jnp.argsort(e_flat)
    e_sorted = e_flat[order]
    counts = jnp.bincount(e_flat, length=N_EXPERTS)
    padded = (counts + MOE_BLOCK - 1) // MOE_BLOCK * MOE_BLOCK
    starts = jnp.cumsum(counts) - counts
    pends = jnp.cumsum(padded)
    pstarts = pends - padded
    rank = jnp.arange(n_asg, dtype=jnp.int32) - starts[e_sorted]
    dest = pstarts[e_sorted] + rank
    n_blocks = -(-n_asg // MOE_BLOCK) + N_EXPERTS
    n_slots = n_blocks * MOE_BLOCK
    slot_tok = jnp.zeros((n_slots,), jnp.int32).at[dest].set(tok_flat[order])
    slot_gate = jnp.zeros((n_slots,), F32).at[dest].set(g_flat[order])
    block_expert = jnp.minimum(
        jnp.searchsorted(pends, jnp.arange(n_blocks) * MOE_BLOCK, side='right'),
        N_EXPERTS - 1).astype(jnp.int32)

    def expert_block(args):
        e, tok, gate = args
        xb = hf[tok]
        gu = xb @ w_gu[e] + b_gu[e]
        g_lin = jnp.minimum(gu[:, :D_FF], SWIGLU_LIMIT)
        u_lin = jnp.clip(gu[:, D_FF:], -SWIGLU_LIMIT, SWIGLU_LIMIT)
        act = g_lin * jax.nn.sigmoid(g_lin * SWIGLU_ALPHA) * (u_lin + 1.0)
        yb = act @ w_down[e] + b_down[e]
        return yb * gate[:, None].astype(yb.dtype)

    yb = lax.map(expert_block, (block_expert,
                                slot_tok.reshape(n_blocks, MOE_BLOCK),
                                slot_gate.reshape(n_blocks, MOE_BLOCK)))
    y = jnp.zeros((n_tok, d), yb.dtype).at[slot_tok].add(yb.reshape(n_slots, d))
    return y.reshape(b, s_len, d).astype(h.dtype)


def setup_inputs(seed: int = 0) -> dict:
    key = jax.random.key(seed)
    ks = jax.random.split(key, 22)
    nrm = lambda k, shape, s: jax.random.normal(k, shape, F32) * s
    L, D, E, F = DEPTH, D_MODEL, N_EXPERTS, D_FF
    x = jax.random.normal(ks[0], (BATCH, SEQ, D), F32)
    c = jax.random.normal(ks[1], (BATCH, D), F32)
    positions = (jnp.arange(SEQ, dtype=jnp.int32)[None, :]
                 + jax.random.randint(ks[2], (BATCH, 1), 0, MAX_POS_OFFSET, dtype=jnp.int32))
    return {
        "x": x,
        "c": c,
        "positions": positions,
        "ada_w": nrm(ks[3], (L, D, 6 * D), 0.5 * D ** -0.5),
        "ada_b": nrm(ks[4], (L, 6 * D), 0.02),
        "norm1_g": 1.0 + nrm(ks[5], (L, D), 0.02),
        "w_in": nrm(ks[6], (L, D, IN_WIDTH), D ** -0.5),
        "q_norm_g": 1.0 + nrm(ks[7], (L, HEAD_DIM), 0.02),
        "k_norm_g": 1.0 + nrm(ks[8], (L, HEAD_DIM), 0.02),
        "attn_sinks": nrm(ks[9], (L, N_Q_HEADS), 0.5),
        "pool_w": nrm(ks[10], (L, N_POOL_GROUPS, POOL_GROUP_DIM, POOL_GROUP_DIM), POOL_GROUP_DIM ** -0.5),
        "pool_b": nrm(ks[11], (L, N_POOL_GROUPS, POOL_GROUP_DIM), 0.02),
        "pool_scale": 1.0 + nrm(ks[12], (L, POOL_WIDTH), 0.05),
        "w_out": nrm(ks[13], (L, MIX_WIDTH, D), MIX_WIDTH ** -0.5),
        "norm2_g": 1.0 + nrm(ks[14], (L, D), 0.02),
        "router_w": nrm(ks[15], (L, D, E), D ** -0.5),
        "router_b": nrm(ks[16], (L, E), 0.01),
        "expert_w_gu": nrm(ks[17], (L, E, D, 2 * F), D ** -0.5),
        "expert_b_gu": nrm(ks[18], (L, E, 2 * F), 0.02),
        "expert_w_down": nrm(ks[19], (L, E, F, D), F ** -0.5),
        "expert_b_down": nrm(ks[20], (L, E, D), 0.02),
    }


def reference(x, c, positions, ada_w, ada_b, norm1_g, w_in, q_norm_g, k_norm_g, attn_sinks,
              pool_w, pool_b, pool_scale, w_out, norm2_g, router_w, router_b,
              expert_w_gu, expert_b_gu, expert_w_down, expert_b_down):
    b, s_len, _ = x.shape
    inv_freq = ROPE_THETA ** (-jnp.arange(0, ROT_DIM, 2, dtype=F32) / ROT_DIM)
    ang = positions.astype(F32)[..., None] * inv_freq
    cos = jnp.cos(ang)[:, :, None, :]
    sin = jnp.sin(ang)[:, :, None, :]
    c_act = jax.nn.silu(c.astype(F32))
    for l in range(DEPTH):
        mod = (c_act @ ada_w[l].astype(F32) + ada_b[l].astype(F32)).astype(x.dtype)
        shift1, scale1, gate1, shift2, scale2, gate2 = jnp.split(mod[:, None, :], 6, axis=-1)

        h = rmsnorm(x, norm1_g[l]) * (1.0 + scale1) + shift1
        proj = h @ w_in[l]
        q, k, v, u = jnp.split(proj, [ATTN_WIDTH, ATTN_WIDTH + KV_WIDTH,
                                      ATTN_WIDTH + 2 * KV_WIDTH], axis=-1)
        q = q.reshape(b, s_len, N_Q_HEADS, HEAD_DIM)
        k = k.reshape(b, s_len, N_KV_HEADS, HEAD_DIM)
        v = v.reshape(b, s_len, N_KV_HEADS, HEAD_DIM)
        q = partial_rope(rmsnorm(q, q_norm_g[l]), cos, sin)
        k = partial_rope(rmsnorm(k, k_norm_g[l]), cos, sin)
        attn_out = sliding_window_attention(q, k, v, attn_sinks[l])
        pool_out = multiscale_pool(u, pool_w[l], pool_b[l], pool_scale[l])
        mixed = jnp.concatenate([attn_out, pool_out.astype(attn_out.dtype)], axis=-1) @ w_out[l]
        x = x + gate1 * mixed

        h2 = rmsnorm(x, norm2_g[l]) * (1.0 + scale2) + shift2
        x = x + gate2 * moe(h2, router_w[l], router_b[l], expert_w_gu[l], expert_b_gu[l],
                            expert_w_down[l], expert_b_down[l])
    return x
```

```python
import math
from contextlib import ExitStack

import numpy as np
import concourse.bass as bass
import concourse.mybir as mybir
from concourse.bass_utils import run_bass_kernel_spmd

F32 = mybir.dt.float32
BF16 = mybir.dt.bfloat16
I32 = mybir.dt.int32
AF = mybir.ActivationFunctionType
ALU = mybir.AluOpType
AX = mybir.AxisListType

D = 1024
NE = 32
HD = 64
EPS = 1e-6
LIMIT = 7.0
ALPHA = 1.702
THETA = 500000.0
TG = 1024
TPG = TG // 128
BLK = 512
SIG_MAX = float(1.0 / (1.0 + math.exp(-ALPHA * LIMIT)))
TWO_PI = 2.0 * math.pi
PI_HI = 6.28125
PI_LO = TWO_PI - PI_HI


class _Op:
    __slots__ = ("eng", "fn", "deps", "dmawait", "semkey", "sem", "val", "signal")


class Sched:
    ENGS = ("pe", "act", "dve", "pool", "sp")
    ROT = 20000

    def __init__(self):
        self.ops = {e: [] for e in self.ENGS}
        self.buf = {}
        self.barrier = set()
        self.barrier_dma = {}
        self.dma_issued = {}
        self.dma_last = {}

    def add(self, eng, fn, reads=(), writes=(), dma=None, extra=()):
        o = _Op()
        o.eng = eng
        o.fn = fn
        o.semkey = dma
        o.sem = None
        o.val = 0
        o.signal = dma is not None
        deps = set(self.barrier)
        deps.update(extra)
        writes = list(writes) + [k for k in reads if k.startswith("ps") and k not in writes]
        for k in reads:
            st = self.buf.get(k)
            if st is not None and st[0] is not None:
                deps.add(st[0])
        for k in writes:
            st = self.buf.get(k)
            if st is not None:
                if st[0] is not None and not st[1]:
                    deps.add(st[0])
                deps.update(st[1])
        o.deps = set()
        o.dmawait = dict(self.barrier_dma)
        for d in deps:
            if d.semkey is not None:
                o.dmawait[d.semkey] = self.dma_issued[d.semkey]
            else:
                o.deps.add(d)
        wset = set(writes)
        for k in reads:
            if k in wset:
                continue
            st = self.buf.get(k)
            if st is None:
                st = self.buf[k] = [None, []]
            st[1].append(o)
        for k in writes:
            self.buf[k] = [o, []]
        self.ops[eng].append(o)
        if dma is not None:
            self.dma_issued[dma] = self.dma_issued.get(dma, 0) + 16
            o.val = self.dma_issued[dma]
            self.dma_last[dma] = o
        return o

    def phase_barrier(self):
        b = set()
        for e in ("pe", "act", "dve", "pool"):
            for o in reversed(self.ops[e]):
                if o.semkey is None:
                    b.add(o)
                    break
        self.barrier_dma = dict(self.dma_issued)
        self.barrier = b

    def emit(self, nc, stack):
        for e in self.ENGS:
            for o in self.ops[e]:
                for d in o.deps:
                    if d.eng == "pe" and o.eng == "pe":
                        continue
                    d.signal = True
        for e in self.ENGS:
            cur = None
            cnt = 0
            n = 0
            for o in self.ops[e]:
                if o.semkey is not None or not o.signal:
                    continue
                if cur is None or cnt >= self.ROT:
                    cur = stack.enter_context(nc.semaphore("s_%s_%d" % (e, n)))
                    n += 1
                    cnt = 0
                cnt += 1
                o.sem = cur
                o.val = cnt
        dsem = {}
        for k in self.dma_issued:
            dsem[k] = stack.enter_context(nc.semaphore("d_%s" % k))
        block = stack.enter_context(nc.Block())

        def run(engname, eng):
            waited = {}
            for o in self.ops[engname]:
                need = {}
                for d in o.deps:
                    if d.eng == "pe" and engname == "pe":
                        continue
                    key = id(d.sem)
                    if key not in need or need[key][1] < d.val:
                        need[key] = (d.sem, d.val)
                for k, v in o.dmawait.items():
                    key = id(dsem[k])
                    if key not in need or need[key][1] < v:
                        need[key] = (dsem[k], v)
                for key, (sem, v) in need.items():
                    if waited.get(key, 0) >= v:
                        continue
                    eng.wait_ge(sem, v)
                    waited[key] = v
                ins = o.fn(eng)
                if o.semkey is not None:
                    ins.then_inc(dsem[o.semkey], 16)
                elif o.signal:
                    ins.then_inc(o.sem, 1)

        @block.tensor
        def _(eng):
            run("pe", eng)

        @block.scalar
        def _(eng):
            run("act", eng)

        @block.vector
        def _(eng):
            run("dve", eng)

        @block.gpsimd
        def _(eng):
            run("pool", eng)

        @block.sync
        def _(eng):
            run("sp", eng)


def _call(name, *args, **kw):
    def fn(e):
        return getattr(e, name)(*args, **kw)
    return fn


class _Nop:
    def then_inc(self, *a):
        return self


def _const_tables():
    ident = np.eye(128, dtype=np.float32)
    kk = np.arange(128)[:, None]
    qq = np.arange(128)[None, :]
    NEG = -30000.0
    mb_cur = np.where(qq >= kk, 0.0, NEG).astype(np.float32)
    mb_prev = np.where(kk > qq, 0.0, NEG).astype(np.float32)
    mb = np.stack([np.tile(mb_prev, (1, 4)), np.tile(mb_cur, (1, 4))], axis=1)
    bt = np.zeros((128, 3, 4, 128), np.float32)
    for g, w in enumerate((2, 4, 8, 16)):
        for t in range(128):
            for j in range(t - w + 1, t + 1):
                if j >= 0:
                    bt[j, 0, g, t] += 1.0 / w
                    bt[j, 2, g, t] += 1.0 / min(t + 1, w)
                else:
                    bt[j + 128, 1, g, t] += 1.0 / w
            bt[t, 0, g, t] -= 1.0
            bt[t, 2, g, t] -= 1.0
    return ident, mb.astype(np.float32), bt.reshape(128, 12 * 128)


def _route_tables(ntok):
    n_slots = ntok * 4 + NE * BLK
    nb = n_slots // BLK
    p = np.arange(128, dtype=np.float32)[:, None]
    ustrict = (np.arange(128)[:, None] < np.arange(128)[None, :]).astype(np.float32)
    iota = np.tile(np.arange(NE, dtype=np.float32)[None, :], (128, 1))
    thr = np.tile((np.arange(16, dtype=np.float32) * BLK)[None, :], (128, 1))
    bvals = np.tile((np.arange(nb, dtype=np.float32) * BLK)[None, :], (128, 1))
    pidx = (np.arange(8, dtype=np.float32)[None, :] * 128 + p).astype(np.float32)
    return ustrict, iota, thr, bvals, pidx


def _inv_freq():
    e = -np.arange(0, 16, 2, dtype=np.float32) / np.float32(16)
    return [float(v) for v in np.power(np.float32(THETA), e).astype(np.float32)]


class _Stop(Exception):
    pass


def build_program(ntok, depth, stop=None):
    nt = ntok // 128
    ng = ntok // TG
    n_slots = ntok * 4 + NE * BLK
    NB = n_slots // BLK
    nc = bass.Bass("TRN2", target_bir_lowering=False)
    S = Sched()
    invf = _inv_freq()

    def din(name, shape, dt=F32):
        return nc.dram_tensor(name, list(shape), dt, kind="ExternalInput").ap()

    x_d = din("x", [ntok, D])
    ccol_d = din("ccol", [128, 8])
    pos_d = din("poscol", [128, nt], I32)
    adaw_d = din("ada_w", [depth, D, 6 * D])
    adab_d = din("adab_col", [128, depth * 48])
    n1g_d = din("n1g_col", [128, depth * 8])
    n2g_d = din("n2g_col", [128, depth * 8])
    win_d = din("w_in", [depth, D, 1280])
    gqk_d = din("gqk", [1, depth * 128])
    sink_d = din("sinks", [1, depth * 8])
    poolw_d = din("pool_w", [depth, 4, 128, 128])
    pb_d = din("pb_col", [128, depth * 4])
    psc_d = din("psc_col", [128, depth * 4])
    wout_d = din("w_out", [depth, D, D])
    rw_d = din("router_w", [depth, D, NE])
    rb_d = din("router_b", [1, depth * NE])
    wgu_d = din("w_gu", [depth, NE, D, 2 * D])
    bgu_d = din("bgu_t", [depth * NE * 128, 16])
    wdn_d = din("w_down", [depth, NE, D, D])
    bdn_d = din("bdn_t", [depth * NE, D])
    cid_d = din("cst_ident", [128, 128])
    cmb_d = din("cst_mb", [128, 2 * 512])
    cbt_d = din("cst_bt", [128, 12 * 128])
    cus_d = din("cst_ustrict", [128, 128])
    cio_d = din("cst_iota", [128, NE])
    cth_d = din("cst_thr", [128, 16])
    cbv_d = din("cst_bvals", [128, NB])
    cpi_d = din("cst_pidx", [128, 8])
    out_d = nc.dram_tensor("out", [ntok, D], F32, kind="ExternalOutput").ap()
    x1_d = nc.dram_tensor("x1buf", [ntok, D], F32, kind="Internal").ap()
    xr_d = nc.dram_tensor("xres", [ntok, D], F32, kind="Internal").ap()
    h2buf_d = nc.dram_tensor("h2buf", [ntok, D], BF16, kind="Internal").ap()
    hslots_d = nc.dram_tensor("hslots", [n_slots, D], BF16, kind="Internal").ap()
    yslots_d = nc.dram_tensor("yslots", [n_slots, D], F32, kind="Internal").ap()
    wgu2d = wgu_d.rearrange("l e d f -> (l e d) f")
    wdn2d = wdn_d.rearrange("l e d f -> (l e d) f")

    with ExitStack() as st:
        def sb(name, shape, dt=F32):
            return st.enter_context(nc.sbuf_tensor("sb_" + name, list(shape), dt))

        AR = sb("AR", [128, 8192], F32)
        WR = sb("WR", [128, 49152], BF16)
        LR = sb("LR", [128, 4096], F32)
        ident_f = sb("ident_f", [128, 128])
        ident_b = sb("ident_b", [128, 128], BF16)
        ones_f = sb("ones_f", [128, 128])
        mb = sb("mb", [128, 2, 512], BF16)
        bt = sb("bt", [128, 12, 128])
        wpool = sb("wpool", [128, depth * 4, 128])
        pb_col = sb("pb_col", [128, depth * 4])
        psc_col = sb("psc_col", [128, depth * 4])
        rw = sb("rw", [128, depth * 8, NE])
        rb_bc = sb("rb_bc", [128, depth * NE])
        gqk_bc = sb("gqk_bc", [128, depth * 128])
        esink = sb("esink", [128, depth * 8])
        cos_t = sb("cos_t", [128, nt, 8])
        sin_t = sb("sin_t", [128, nt, 8])
        ccol = sb("ccol", [128, 8])
        cact = sb("cact", [128, 8])
        adab = sb("adab", [128, depth * 48])
        n1g = sb("n1g", [128, depth * 8])
        n2g = sb("n2g", [128, depth * 8])
        modT = sb("modT", [128, depth * 48])
        A1 = sb("A1", [128, depth * 8])
        A2 = sb("A2", [128, depth * 8])
        G1bc = LR[:, 0:1024]
        G2bc = sb("G2bc", [128, D])
        gtmp = sb("gtmp", [128, 128])
        A2bc = LR[:, 1024:2048]
        S2bc = LR[:, 2048:3072]
        ustrict = sb("ustrict", [128, 128])
        iotaE = sb("iotaE", [128, NE])
        thr512 = sb("thr512", [128, 16])
        bvals = sb("bvals", [128, NB])
        pidx = sb("pidx", [128, 8])
        cum = sb("cum", [128, NE])
        rt_pad = sb("rt_pad", [128, NE])
        rt_pend = sb("rt_pend", [128, NE])
        rt_pst = sb("rt_pst", [128, NE])
        ebf = sb("ebf", [128, NB])
        rank4 = sb("rank4", [128, nt, 4])
        e4 = sb("e4", [128, nt, 4])
        G4 = sb("G4", [128, nt, 4])
        dest_i = sb("dest_i", [128, nt, 4], I32)
        IW_i = sb("IW_i", [128, NB, 8], I32)
        Ib_i = sb("Ib_i", [128, NB], I32)
        Id_i = sb("Id_i", [128, NB], I32)
        bgub = [sb("bgub%d" % i, [128, 16]) for i in range(2)]
        bgs = [sb("bgs%d" % i, [128, 8]) for i in range(2)]
        bu1 = [sb("bu1%d" % i, [128, 8]) for i in range(2)]
        bdbc = [sb("bdbc%d" % i, [128, D], BF16) for i in range(2)]
        kT = [sb("kT%d" % i, [128, 128], BF16) for i in range(2)]
        vaug = [sb("vaug%d" % i, [128, 2, 65], BF16) for i in range(2)]
        usb = [LR[:, 3072 + i * 512: 3072 + (i + 1) * 512] for i in range(2)]
        h2Tb = [sb("h2Tb%d" % i, [128, 8, BLK], BF16) for i in range(2)]
        act_sb = [LR[:, 2048 + i * 1024: 2048 + (i + 1) * 1024].bitcast(BF16).rearrange("p (j t) -> p j t", j=4)
                  for i in range(2)]
        sg_t = LR[:, 0:512]
        gl_t = LR[:, 512:1024]
        t1_t = LR[:, 1024:1536]
        aa_t = LR[:, 1536:2048]
        sm = sb("sm", [128, 256])
        posf = sb("posf", [128, nt])
        cst = sb("cst", [128, 8])
        ps = [st.enter_context(nc.psum_tensor("ps%d" % i, [128, 512], F32)) for i in range(8)]

        def arv(off, n):
            return AR[:, off:off + n]

        xt2 = [arv(0, 1024), WR[:, 25088:27136].bitcast(F32)]
        xn = arv(1024, 1024)
        x1 = arv(2048, 1024)
        xn2 = arv(3072, 1024)
        h2Tf = arv(4096, 1024).rearrange("p (k t) -> p k t", k=8)
        qn = arv(5120, 640)
        attn = arv(5760, 512)
        pooledT = arv(6272, 512)
        sq = arv(6784, 640)
        rtmp = arv(7424, 320).rearrange("p (a h d) -> p a h d", a=4, h=10)
        lg = arv(7744, 32)
        msk = arv(7776, 32)
        exv = arv(7808, 32)
        exm = arv(7840, 32)
        sel4 = arv(7872, 128).rearrange("p (k e) -> p k e", k=4)
        prod4 = arv(8000, 128).rearrange("p (k e) -> p k e", k=4)
        trig = AR[:, 0:4 * nt * 8].rearrange("p (a n) -> p a n", a=4)
        trig_i = AR[:, 4096:4096 + nt * 8].bitcast(I32)
        big = AR[:, 0:max(NB * NE, NE * 16, nt * 4, NB * 8)]
        ystage = AR[:, 0:4096].rearrange("p (t d) -> p t d", t=4)
        yk = [arv(k * 1024, 1024) for k in range(4)]
        ex1 = arv(4096, 1024)
        ea = arv(5120, 1024)
        adw = [arv(0, 4096).rearrange("p (k f) -> p k f", k=8),
               arv(4096, 4096).rearrange("p (k f) -> p k f", k=8)]

        w_in = WR[:, 0:10240].rearrange("p (k f) -> p k f", k=8)
        w_out = WR[:, 10240:18432].rearrange("p (k f) -> p k f", k=8)
        junk = WR[:, 18432:19456]
        hT = WR[:, 19456:20480].rearrange("p (k t) -> p k t", k=8)
        qT = WR[:, 20480:20992]
        PT = [[WR[:, 20992 + (b * 2 + h) * 512: 20992 + (b * 2 + h + 1) * 512] for h in range(2)]
              for b in range(2)]
        mixT = WR[:, 23040:24064].rearrange("p (k t) -> p k t", k=8)
        h2row = WR[:, 24064:25088]
        Wgu = [WR[:, p * 16384:(p + 1) * 16384].rearrange("p (k f) -> p k f", k=8) for p in range(2)]
        Wd = [WR[:, 32768 + q * 8192: 32768 + (q + 1) * 8192].rearrange("p (j d) -> p j d", j=8) for q in range(2)]
        hs = [WR[:, q * 1024:(q + 1) * 1024] for q in range(2)]
        hrows = [arv(4096 + q * 2048, 2048).bitcast(BF16).rearrange("p (t d) -> p t d", t=4) for q in range(2)]

        def smv(i, n=1):
            return sm[:, i:i + n]

        def dve(fn, r=(), w=()):
            return S.add("dve", fn, r, w)

        def actop(fn, r=(), w=()):
            return S.add("act", fn, r, w)

        def pe(fn, r=(), w=()):
            return S.add("pe", fn, r, w)

        def dma_sp(key, out, in_, r=(), w=()):
            return S.add("sp", _call("dma_start", out=out, in_=in_), r, w, dma=key)

        def dma_pool(key, out, in_, r=(), w=()):
            return S.add("pool", _call("dma_start", out=out, in_=in_), r, w, dma=key)

        def mm_group(specs):
            def fn(e):
                ins = None
                for (o, l, r_, s0, s1) in specs:
                    ins = e.matmul(o, l, r_, start=s0, stop=s1)
                return ins
            return fn

        def tr_group_b(specs):
            def fn(e):
                ins = None
                for (o, i_) in specs:
                    ins = e.transpose(out=o, in_=i_, identity=ident_b[:])
                return ins
            return fn

        def tr_group(specs):
            def fn(e):
                ins = None
                for (o, i_) in specs:
                    ins = e.transpose(out=o, in_=i_, identity=ident_f[:])
                return ins
            return fn

        dma_sp("c0", ident_f[:], cid_d, w=["ident_f"])
        dma_sp("c0", bt[:], cbt_d.rearrange("p (a t) -> p a t", a=12), w=["bt"])
        dma_sp("c0", wpool[:], poolw_d.rearrange("l g c d -> c (l g) d"), w=["wpool"])
        dma_sp("c0", pb_col[:], pb_d, w=["pb_col"])
        dma_sp("c0", psc_col[:], psc_d, w=["psc_col"])
        dma_sp("c0", rw[:], rw_d.rearrange("l (k p) e -> p (l k) e", p=128), w=["rw"])
        dma_sp("c0", rb_bc[:], rb_d[0:1, :].broadcast_to([128, depth * NE]), w=["rb_bc"])
        dma_sp("c0", gqk_bc[:], gqk_d[0:1, :].broadcast_to([128, depth * 128]), w=["gqk_bc"])
        dma_sp("c0", esink[:], sink_d[0:1, :].broadcast_to([128, depth * 8]), w=["esink"])
        dma_sp("c0", ccol[:], ccol_d, w=["ccol"])
        dma_sp("c0", adab[:], adab_d, w=["adab"])
        dma_sp("c0", n1g[:], n1g_d, w=["n1g"])
        dma_sp("c0", n2g[:], n2g_d, w=["n2g"])
        dma_sp("c0", ustrict[:], cus_d, w=["ustrict"])
        dma_sp("c0", iotaE[:], cio_d, w=["iotaE"])
        dma_sp("c0", thr512[:], cth_d, w=["thr512"])
        dma_sp("c0", bvals[:], cbv_d, w=["bvals"])
        dma_sp("c0", pidx[:], cpi_d, w=["pidx"])
        dma_sp("c0", trig_i[:, 0:nt], pos_d, w=["trig_i"])
        dma_pool("c1", mb[:], cmb_d.rearrange("p (a t) -> p a t", a=2), w=["mb"])

        dve(_call("memset", ones_f[:], 1.0), w=["ones_f"])
        dve(_call("memset", cst[:, 0:1], EPS), w=["cst0"])
        dve(_call("memset", cst[:, 1:2], math.pi / 2.0), w=["cst1"])
        for i in range(2):
            dve(_call("memset", vaug[i][:], 1.0), w=["vaug%d" % i])
            dve(_call("memset", usb[i][:], 0.0), w=["usb%d" % i])
        dve(_call("tensor_copy", out=ident_b[:], in_=ident_f[:]), r=["ident_f"], w=["ident_b"])
        actop(_call("activation", out=esink[:], in_=esink[:], func=AF.Exp), r=["esink"], w=["esink"])
        dve(_call("memset", cum[:], 0.0), w=["cum"])
        dve(_call("memset", WR[:, 0:8192], 0.0), w=["zsrc"])
        zsrc = WR[:, 0:8192].rearrange("p (a d) -> p a d", d=D)
        for z0 in range(0, n_slots, 1024):
            dma_pool("zf", hslots_d[z0:z0 + 1024, :].rearrange("(p a) d -> p a d", p=128), zsrc, r=["zsrc"])

        dve(_call("tensor_copy", out=posf[:], in_=trig_i[:, 0:nt]), r=["trig_i"], w=["posf"])
        ang = trig[:, 0, :]
        kf = trig[:, 1, :]
        rr = trig[:, 2, :]
        t3 = trig[:, 3, :]
        ang3 = ang.rearrange("p (t j) -> p t j", j=8)
        for j in range(8):
            dve(_call("tensor_scalar", out=ang3[:, :, j], in0=posf[:], scalar1=invf[j], scalar2=None,
                                               op0=ALU.mult), r=["posf"], w=["ang"])
        dve(_call("tensor_scalar", out=kf, in0=ang, scalar1=1.0 / TWO_PI, scalar2=0.5, op0=ALU.mult,
                                      op1=ALU.add), r=["ang"], w=["kf"])
        dve(_call("tensor_copy", out=trig_i[:], in_=kf), r=["kf"], w=["trig_i"])
        dve(_call("tensor_copy", out=kf, in_=trig_i[:]), r=["trig_i"], w=["kf"])
        dve(_call("scalar_tensor_tensor", out=rr, in0=kf, scalar=-PI_HI, in1=ang, op0=ALU.mult, op1=ALU.add),
            r=["kf", "ang"], w=["rr"])
        dve(_call("scalar_tensor_tensor", out=rr, in0=kf, scalar=-PI_LO, in1=rr, op0=ALU.mult, op1=ALU.add),
            r=["kf", "rr"], w=["rr"])
        dve(_call("tensor_scalar", out=t3, in0=rr, scalar1=math.pi, scalar2=-TWO_PI, op0=ALU.is_gt,
                                      op1=ALU.mult), r=["rr"], w=["t3"])
        dve(_call("tensor_tensor", out=rr, in0=rr, in1=t3, op=ALU.add), r=["rr", "t3"], w=["rr"])
        dve(_call("tensor_scalar", out=t3, in0=rr, scalar1=-math.pi, scalar2=TWO_PI, op0=ALU.is_lt,
                                      op1=ALU.mult), r=["rr"], w=["t3"])
        dve(_call("tensor_tensor", out=rr, in0=rr, in1=t3, op=ALU.add), r=["rr", "t3"], w=["rr"])
        dve(_call("tensor_scalar", out=rr, in0=rr, scalar1=math.pi, scalar2=-math.pi, op0=ALU.min,
                                      op1=ALU.max), r=["rr"], w=["rr"])
        dve(_call("scalar_tensor_tensor", out=t3, in0=rr, scalar=-1.0, in1=rr, op0=ALU.mult, op1=ALU.max), r=["rr"], w=["t3"])
        actop(_call("activation", out=sin_t[:].rearrange("p t j -> p (t j)"), in_=rr, func=AF.Sin),
              r=["rr"], w=["sin_t"])
        actop(_call("activation", out=cos_t[:].rearrange("p t j -> p (t j)"), in_=t3, func=AF.Sin,
                                     scale=-1.0, bias=cst[:, 1:2]), r=["t3", "cst1"], w=["cos_t"])

        S.phase_barrier()
        actop(_call("activation", out=cact[:], in_=ccol[:], func=AF.Silu), r=["ccol"], w=["cact"])
        npiece = 0
        for l in range(depth):
            for n in range(12):
                par = npiece % 2
                npiece += 1
                src = adaw_d[l].rearrange("(k p) f -> p k f", p=128)[:, :, n * 512:(n + 1) * 512]
                dma_sp("adw%d" % par, adw[par], src, w=["adw%d" % par])
                specs = []
                for jj in range(4):
                    col = l * 48 + n * 4 + jj
                    for k in range(8):
                        specs.append((ps[0][:, col:col + 1], adw[par][:, k, jj * 128:(jj + 1) * 128],
                                      cact[:, k:k + 1], k == 0, k == 7))
                pe(mm_group(specs), r=["adw%d" % par, "cact"], w=["ps0"])
        dve(_call("tensor_tensor", out=modT[:], in0=ps[0][:, 0:depth * 48], in1=adab[:], op=ALU.add),
            r=["ps0", "adab"], w=["modT"])
        modT3 = modT[:].rearrange("p (l j) -> p l j", j=48)
        dve(_call("scalar_tensor_tensor", out=A1[:].rearrange("p (l k) -> p l k", k=8), in0=modT3[:, :, 8:16],
                                             scalar=1.0, in1=n1g[:].rearrange("p (l k) -> p l k", k=8),
                                             op0=ALU.add, op1=ALU.mult), r=["modT", "n1g"], w=["A1"])
        dve(_call("scalar_tensor_tensor", out=A2[:].rearrange("p (l k) -> p l k", k=8), in0=modT3[:, :, 32:40],
                                             scalar=1.0, in1=n2g[:].rearrange("p (l k) -> p l k", k=8),
                                             op0=ALU.add, op1=ALU.mult), r=["modT", "n2g"], w=["A2"])

        def S1c(l, k):
            return modT[:, l * 48 + k: l * 48 + k + 1]

        def S2c(l, k):
            return modT[:, l * 48 + 24 + k: l * 48 + 24 + k + 1]

        S.phase_barrier()
        if stop == "setup":
            depth_run = 0
        else:
            depth_run = depth

        wslot = [0]
        def chk(name):
            if stop == name:
                raise _Stop()

        try:
            for l in range(depth_run):
                src_d = x_d if l == 0 else xr_d
                dst_d = out_d if l == depth - 1 else xr_d
                for (gt_tile, off, nm) in ((G1bc, 16, "G1bc"), (G2bc, 40, "G2bc"), (S2bc, 24, "S2bc"), (A2bc, -1, "A2bc")):
                    for half in range(2):
                        for kk in range(4):
                            k = half * 4 + kk
                            colap = (A2[:, l * 8 + k: l * 8 + k + 1] if off < 0 else
                                     modT[:, l * 48 + off + k: l * 48 + off + k + 1])
                            dve(_call("tensor_scalar",
                                out=gtmp[:], in0=ones_f[:], scalar1=colap,
                                scalar2=None, op0=ALU.mult), r=["ones_f", "modT", "A2"], w=["gtmp"])
                            pe(mm_group([(ps[1][:, kk * 128:(kk + 1) * 128], gtmp[:], ident_f[:], True, True)]),
                               r=["gtmp", "ident_f"], w=["ps1"])
                        dve(_call("tensor_copy",
                            out=gt_tile[:, half * 512:(half + 1) * 512], in_=ps[1][:]), r=["ps1"], w=[nm])
                S.phase_barrier()
                chk("gates")

                for g in range(ng):
                    wsrc = win_d[l].rearrange("(k p) f -> p k f", p=128)
                    for (c0, c1) in ((0, 512), (512, 1024), (1024, 1280)):
                        if g == 0:
                            dma_pool("win", w_in[:, :, c0:c1], wsrc[:, :, c0:c1], w=["w_in%d" % c0])
                    wsrc2 = wout_d[l].rearrange("(k p) f -> p k f", p=128)
                    for (c0, c1) in ((0, 512), (512, 1024)):
                        if g == 0:
                            dma_pool("wout", w_out[:, :, c0:c1], wsrc2[:, :, c0:c1], w=["w_out%d" % c0])

                    for tl in range(TPG):
                        i = g * TPG + tl
                        par = i % 2
                        r0 = i * 128
                        if i == 0:
                            dma_sp("xt0", xt2[0], src_d[0:128, :], w=["xt0"])
                        if i + 1 < nt:
                            dma_sp("xt%d" % (1 - par), xt2[1 - par], src_d[r0 + 128:r0 + 256, :], w=["xt%d" % (1 - par)])
                        xt = xt2[par]
                        xtk = "xt%d" % par
                        actop(_call("activation", out=junk, in_=xt, func=AF.Square, accum_out=smv(0)),
                              r=[xtk], w=["junk", "ss1"])
                        actop(_call("activation", out=smv(1), in_=smv(0), func=AF.Sqrt, scale=1.0 / D,
                                                     bias=cst[:, 0:1]), r=["ss1", "cst0"], w=["sd1"])
                        dve(_call("reciprocal", out=smv(2), in_=smv(1)), r=["sd1"], w=["rstd1"])
                        actop(_call("activation", out=xn, in_=xt, func=AF.Identity, scale=smv(2)),
                              r=[xtk, "rstd1"], w=["xn"])
                        for b in range(2):
                            pe(tr_group([(ps[b][:, j * 128:(j + 1) * 128], xn[:, (b * 4 + j) * 128:(b * 4 + j + 1) * 128])
                                         for j in range(4)]), r=["xn", "ident_f"], w=["ps%d" % b])
                            for j in range(4):
                                k = b * 4 + j
                                if j % 2 == 0:
                                    dve(_call("tensor_scalar",
                                        out=hT[:, k, :], in0=ps[b][:, j * 128:(j + 1) * 128],
                                        scalar1=A1[:, l * 8 + k: l * 8 + k + 1], scalar2=S1c(l, k),
                                        op0=ALU.mult, op1=ALU.add), r=["ps%d" % b, "A1", "modT"], w=["hT%d" % k])
                                else:
                                    actop(_call("activation",
                                        out=hT[:, k, :], in_=ps[b][:, j * 128:(j + 1) * 128], func=AF.Identity,
                                        scale=A1[:, l * 8 + k: l * 8 + k + 1], bias=S1c(l, k)),
                                        r=["ps%d" % b, "A1", "modT"], w=["hT%d" % k])
                        specs = []
                        for k in range(8):
                            specs.append((ps[2][:, :], hT[:, k, :], w_in[:, k, 0:512], k == 0, k == 7))
                            specs.append((ps[3][:, 0:256], hT[:, k, :], w_in[:, k, 512:768], k == 0, k == 7))
                            specs.append((ps[4][:, :], hT[:, k, :], w_in[:, k, 768:1280], k == 0, k == 7))
                        pe(mm_group(specs), r=["hT%d" % k for k in range(8)] + ["w_in0", "w_in512", "w_in1024"], w=["ps2", "ps3", "ps4"])
                        dve(_call("tensor_copy",
                            out=vaug[par][:, :, 0:64], in_=ps[3][:, 128:256].rearrange("p (h d) -> p h d", h=2)),
                            r=["ps3"], w=["vaug%d" % par])
                        actop(_call("activation", out=usb[par][:], in_=ps[4][:], func=AF.Identity),
                              r=["ps4"], w=["usb%d" % par])
                        actop(_call("activation", out=sq[:, 0:512], in_=ps[2][:], func=AF.Square), r=["ps2"], w=["sqq"])
                        actop(_call("activation", out=sq[:, 512:640], in_=ps[3][:, 0:128], func=AF.Square),
                              r=["ps3"], w=["sqk"])
                        dve(_call("tensor_reduce", out=smv(8, 10), in_=sq.rearrange("p (h d) -> p h d", d=64),
                                                      axis=AX.X, op=ALU.add), r=["sqq", "sqk"], w=["ssq"])
                        actop(_call("activation", out=smv(20, 10), in_=smv(8, 10), func=AF.Sqrt, scale=1.0 / HD,
                                                     bias=cst[:, 0:1]), r=["ssq", "cst0"], w=["sdq"])
                        dve(_call("reciprocal", out=smv(32, 10), in_=smv(20, 10)), r=["sdq"], w=["rq"])
                        qn3 = qn.rearrange("p (h d) -> p h d", d=64)
                        dve(_call("tensor_tensor",
                            out=qn[:, 0:512].rearrange("p (g hk d) -> p hk g d", g=4, hk=2),
                            in0=ps[2][:].rearrange("p (hk g d) -> p hk g d", hk=2, g=4),
                            in1=smv(32, 8).rearrange("p (hk g) -> p hk g", hk=2).unsqueeze(3).broadcast_to([128, 2, 4, 64]),
                            op=ALU.mult), r=["ps2", "rq"], w=["qnq"])
                        dve(_call("tensor_tensor",
                            out=qn3[:, 8:10, :], in0=ps[3][:, 0:128].rearrange("p (h d) -> p h d", d=64),
                            in1=smv(40, 2).unsqueeze(2).broadcast_to([128, 2, 64]), op=ALU.mult),
                            r=["ps3", "rq"], w=["qnk"])
                        dve(_call("tensor_tensor",
                            out=qn3[:, 0:8, :], in0=qn3[:, 0:8, :],
                            in1=gqk_bc[:, l * 128: l * 128 + 64].unsqueeze(1).broadcast_to([128, 8, 64]), op=ALU.mult),
                            r=["qnq", "gqk_bc"], w=["qnq"])
                        dve(_call("tensor_tensor",
                            out=qn3[:, 8:10, :], in0=qn3[:, 8:10, :],
                            in1=gqk_bc[:, l * 128 + 64: l * 128 + 128].unsqueeze(1).broadcast_to([128, 2, 64]),
                            op=ALU.mult), r=["qnk", "gqk_bc"], w=["qnk"])
                        cosb = cos_t[:, i, :].unsqueeze(1).broadcast_to([128, 10, 8])
                        sinb = sin_t[:, i, :].unsqueeze(1).broadcast_to([128, 10, 8])
                        t1v = qn3[:, :, 0:8]
                        t2v = qn3[:, :, 8:16]
                        dve(_call("tensor_tensor", out=rtmp[:, 0], in0=t1v, in1=cosb, op=ALU.mult),
                            r=["qnq", "qnk", "cos_t"], w=["rt0"])
                        dve(_call("tensor_tensor", out=rtmp[:, 1], in0=t2v, in1=sinb, op=ALU.mult),
                            r=["qnq", "qnk", "sin_t"], w=["rt1"])
                        dve(_call("tensor_tensor", out=rtmp[:, 2], in0=t2v, in1=cosb, op=ALU.mult),
                            r=["qnq", "qnk", "cos_t"], w=["rt2"])
                        dve(_call("tensor_tensor", out=rtmp[:, 3], in0=t1v, in1=sinb, op=ALU.mult),
                            r=["qnq", "qnk", "sin_t"], w=["rt3"])
                        dve(_call("tensor_tensor", out=t1v, in0=rtmp[:, 0], in1=rtmp[:, 1], op=ALU.subtract),
                            r=["rt0", "rt1"], w=["qnq", "qnk"])
                        dve(_call("tensor_tensor", out=t2v, in0=rtmp[:, 2], in1=rtmp[:, 3], op=ALU.add),
                            r=["rt2", "rt3"], w=["qnq", "qnk"])
                        pe(tr_group([(ps[5][:, gg * 128:(gg + 1) * 128], qn[:, gg * 128:(gg + 1) * 128]) for gg in range(4)]),
                           r=["qnq", "qnk", "ident_f"], w=["ps5"])
                        pe(tr_group([(ps[6][:, 0:128], qn[:, 512:640])]), r=["qnq", "qnk", "ident_f"], w=["ps6"])
                        dve(_call("tensor_copy", out=qT, in_=ps[5][:]), r=["ps5"], w=["qT"])
                        actop(_call("activation", out=kT[par][:], in_=ps[6][:, 0:128], func=AF.Identity),
                              r=["ps6"], w=["kT%d" % par])
                        blocks = ([0] if i > 0 else []) + [1]
                        sbank = {(0, 0): 7, (0, 1): 0, (1, 0): 1, (1, 1): 6}
                        for bk in blocks:
                            kpar = par if bk == 1 else 1 - par
                            for h in range(2):
                                bnk = sbank[(bk, h)]
                                pe(mm_group([
                                    (ps[bnk][:, :], kT[kpar][h * 64:(h + 1) * 64, :], qT[h * 64:(h + 1) * 64, :], True, False),
                                    (ps[bnk][:, :], ident_b[:], mb[:, bk, :], False, True)]),
                                   r=["kT%d" % kpar, "qT", "ident_b", "mb"], w=["ps%d" % bnk])
                                actop(_call("activation",
                                    out=PT[bk][h], in_=ps[bnk][:], func=AF.Exp, scale=HD ** -0.5),
                                    r=["ps%d" % bnk], w=["PT%d%d" % (bk, h)])
                        for h in range(2):
                            specs = []
                            for gg in range(4):
                                first = True
                                for bk in blocks:
                                    kpar = par if bk == 1 else 1 - par
                                    specs.append((ps[2 + h][:, gg * 65:(gg + 1) * 65],
                                                  PT[bk][h][:, gg * 128:(gg + 1) * 128], vaug[kpar][:, h, :],
                                                  first, bk == 1))
                                    first = False
                            pe(mm_group(specs), r=["PT%d%d" % (bk, h) for bk in blocks] + ["vaug0", "vaug1"],
                               w=["ps%d" % (2 + h)])
                            O3 = ps[2 + h][:, 0:260].rearrange("p (g d) -> p g d", d=65)
                            dve(_call("tensor_tensor",
                                out=smv(48 + h * 4, 4), in0=O3[:, :, 64], in1=esink[:, l * 8 + h * 4: l * 8 + h * 4 + 4],
                                op=ALU.add), r=["ps%d" % (2 + h), "esink"], w=["den%d" % h])
                            dve(_call("reciprocal", out=smv(56 + h * 4, 4), in_=smv(48 + h * 4, 4)),
                                r=["den%d" % h], w=["rden%d" % h])
                            dve(_call("tensor_tensor",
                                out=attn[:, h * 256:(h + 1) * 256].rearrange("p (g d) -> p g d", d=64),
                                in0=O3[:, :, 0:64], in1=smv(56 + h * 4, 4).unsqueeze(2).broadcast_to([128, 4, 64]),
                                op=ALU.mult), r=["ps%d" % (2 + h), "rden%d" % h], w=["attn%d" % h])
                        pe(tr_group([(ps[5][:, j * 128:(j + 1) * 128], attn[:, j * 128:(j + 1) * 128]) for j in range(4)]),
                           r=["attn0", "attn1", "ident_f"], w=["ps5"])
                        actop(_call("activation", out=mixT[:, 0:4, :], in_=ps[5][:].rearrange("p (k t) -> p k t", k=4),
                                                     func=AF.Identity), r=["ps5"], w=["mixTa"])
                        specs = []
                        for gg in range(4):
                            if i == 0:
                                specs.append((ps[4][:, gg * 128:(gg + 1) * 128], usb[par][:, gg * 128:(gg + 1) * 128],
                                              bt[:, 8 + gg, :], True, True))
                            else:
                                specs.append((ps[4][:, gg * 128:(gg + 1) * 128], usb[par][:, gg * 128:(gg + 1) * 128],
                                              bt[:, gg, :], True, False))
                                specs.append((ps[4][:, gg * 128:(gg + 1) * 128], usb[1 - par][:, gg * 128:(gg + 1) * 128],
                                              bt[:, 4 + gg, :], False, True))
                        pe(mm_group(specs), r=["usb0", "usb1", "bt"], w=["ps4"])
                        dve(_call("tensor_copy", out=pooledT, in_=ps[4][:]), r=["ps4"], w=["pooledT"])
                        pe(mm_group([(ps[7][:, gg * 128:(gg + 1) * 128], wpool[:, l * 4 + gg, :],
                                      pooledT[:, gg * 128:(gg + 1) * 128], True, True) for gg in range(4)]),
                           r=["pooledT", "wpool"], w=["ps7"])
                        for gg in range(4):
                            dve(_call("tensor_scalar",
                                out=mixT[:, 4 + gg, :], in0=ps[7][:, gg * 128:(gg + 1) * 128],
                                scalar1=pb_col[:, l * 4 + gg: l * 4 + gg + 1], scalar2=psc_col[:, l * 4 + gg: l * 4 + gg + 1],
                                op0=ALU.add, op1=ALU.mult), r=["ps7", "pb_col", "psc_col"], w=["mixTb%d" % gg])
                        for n in range(2):
                            pe(mm_group([(ps[n][:, :], mixT[:, k, :], w_out[:, k, n * 512:(n + 1) * 512], k == 0, k == 7)
                                         for k in range(8)]),
                               r=["mixTa", "mixTb0", "mixTb1", "mixTb2", "mixTb3", "w_out0", "w_out512"], w=["ps%d" % n])
                            dve(_call("tensor_tensor", out=x1[:, n * 512:(n + 1) * 512], in0=ps[n][:],
                                                               in1=G1bc[:, n * 512:(n + 1) * 512], op=ALU.mult),
                                r=["ps%d" % n, "G1bc"], w=["x1_%d" % n])
                            dve(_call("tensor_tensor", out=x1[:, n * 512:(n + 1) * 512],
                                                               in0=x1[:, n * 512:(n + 1) * 512],
                                                               in1=xt[:, n * 512:(n + 1) * 512], op=ALU.add),
                                r=["x1_%d" % n, xtk], w=["x1_%d" % n])
                        dma_sp("x1st", x1_d[r0:r0 + 128, :], x1, r=["x1_0", "x1_1"])
                        actop(_call("activation", out=junk, in_=x1, func=AF.Square, accum_out=smv(4)),
                              r=["x1_0", "x1_1"], w=["junk", "ss2"])
                        actop(_call("activation", out=smv(5), in_=smv(4), func=AF.Sqrt, scale=1.0 / D,
                                                     bias=cst[:, 0:1]), r=["ss2", "cst0"], w=["sd2"])
                        dve(_call("reciprocal", out=smv(6), in_=smv(5)), r=["sd2"], w=["rstd2"])
                        dve(_call("tensor_scalar", out=xn2, in0=x1, scalar1=smv(6), scalar2=None, op0=ALU.mult),
                            r=["x1_0", "x1_1", "rstd2"], w=["xn2"])
                        for b in range(2):
                            bnk = 5 + b
                            pe(tr_group([(ps[bnk][:, j * 128:(j + 1) * 128], xn2[:, (b * 4 + j) * 128:(b * 4 + j + 1) * 128])
                                         for j in range(4)]), r=["xn2", "ident_f"], w=["ps%d" % bnk])
                            for j in range(4):
                                k = b * 4 + j
                                if j % 2 == 1:
                                    dve(_call("tensor_scalar",
                                        out=h2Tf[:, k, :], in0=ps[bnk][:, j * 128:(j + 1) * 128],
                                        scalar1=A2[:, l * 8 + k: l * 8 + k + 1], scalar2=S2c(l, k),
                                        op0=ALU.mult, op1=ALU.add), r=["ps%d" % bnk, "A2", "modT"], w=["h2Tf%d" % k])
                                else:
                                    actop(_call("activation",
                                        out=h2Tf[:, k, :], in_=ps[bnk][:, j * 128:(j + 1) * 128], func=AF.Identity,
                                        scale=A2[:, l * 8 + k: l * 8 + k + 1], bias=S2c(l, k)),
                                        r=["ps%d" % bnk, "A2", "modT"], w=["h2Tf%d" % k])

                        dve(_call("tensor_tensor", out=xn, in0=xn2, in1=A2bc[:], op=ALU.mult),
                            r=["xn2", "A2bc", "xn"], w=["xn"])
                        dve(_call("tensor_tensor", out=h2row, in0=xn, in1=S2bc[:], op=ALU.add),
                            r=["xn", "S2bc"], w=["h2row"])
                        dma_sp("h2st", h2buf_d[r0:r0 + 128, :], h2row, r=["h2row"])
                        pe(mm_group([(ps[7][:, 0:NE], h2Tf[:, k, :], rw[:, l * 8 + k, :], k == 0, k == 7) for k in range(8)]),
                           r=["h2Tf%d" % k for k in range(8)] + ["rw"], w=["ps7"])
                        dve(_call("tensor_tensor", out=lg, in0=ps[7][:, 0:NE], in1=rb_bc[:, l * NE:(l + 1) * NE],
                                  op=ALU.add), r=["ps7", "rb_bc"], w=["lg"])
                        dve(_call("max", out=smv(64, 8), in_=lg), r=["lg"], w=["top8"])
                        dve(_call("tensor_scalar", out=smv(72), in0=smv(64), scalar1=-1.0, scalar2=None, op0=ALU.mult),
                            r=["top8"], w=["nmax"])
                        dve(_call("tensor_scalar", out=msk, in0=lg, scalar1=smv(67), scalar2=None, op0=ALU.is_ge),
                            r=["lg", "top8"], w=["msk"])
                        actop(_call("activation", out=smv(80, 4), in_=smv(64, 4), func=AF.Exp, bias=smv(72)),
                              r=["top8", "nmax"], w=["e4x"])
                        dve(_call("tensor_reduce", out=smv(84), in_=smv(80, 4), axis=AX.X, op=ALU.add), r=["e4x"], w=["gden"])
                        dve(_call("reciprocal", out=smv(85), in_=smv(84)), r=["gden"], w=["grd"])
                        dve(_call("tensor_scalar", out=G4[:, i, :], in0=smv(80, 4), scalar1=smv(85), scalar2=None,
                                  op0=ALU.mult), r=["e4x", "grd"], w=["G4"])
                        pe(mm_group([(ps[7][:, 64:96], ustrict[:], msk, True, True),
                                     (ps[7][:, 96:128], ones_f[:], msk, True, True)]),
                           r=["msk", "ustrict", "ones_f"], w=["ps7"])
                        dve(_call("tensor_tensor", out=exv, in0=ps[7][:, 64:96], in1=cum[:], op=ALU.add),
                            r=["ps7", "cum"], w=["rankt"])
                        dve(_call("tensor_tensor", out=cum[:], in0=cum[:], in1=ps[7][:, 96:128], op=ALU.add),
                            r=["ps7", "cum"], w=["cum"])
                        lgb = lg.unsqueeze(1).broadcast_to([128, 4, NE])
                        t8b = smv(64, 4).unsqueeze(2).broadcast_to([128, 4, NE])
                        dve(_call("tensor_tensor", out=sel4, in0=lgb, in1=t8b, op=ALU.is_equal), r=["lg", "top8"], w=["sel4"])
                        dve(_call("tensor_tensor", out=prod4, in0=sel4, in1=exv.unsqueeze(1).broadcast_to([128, 4, NE]),
                                  op=ALU.mult), r=["sel4", "rankt"], w=["prod4"])
                        dve(_call("tensor_reduce", out=rank4[:, i, :], in_=prod4, axis=AX.X, op=ALU.add), r=["prod4"], w=["rank4"])
                        dve(_call("tensor_tensor", out=prod4, in0=sel4, in1=iotaE[:].unsqueeze(1).broadcast_to([128, 4, NE]),
                                  op=ALU.mult), r=["sel4", "iotaE"], w=["prod4"])
                        dve(_call("tensor_reduce", out=e4[:, i, :], in_=prod4, axis=AX.X, op=ALU.add), r=["prod4"], w=["e4"])
                        if i == 0:
                            chk("tile0")
                S.phase_barrier()
                chk("mixer")

                cmp3 = big[:, 0:NE * 16].rearrange("p (e m) -> p e m", m=16)
                dve(_call("tensor_tensor", out=cmp3, in0=cum[:].unsqueeze(2).broadcast_to([128, NE, 16]),
                          in1=thr512[:].unsqueeze(1).broadcast_to([128, NE, 16]), op=ALU.is_gt),
                    r=["cum", "thr512"], w=["big"])
                dve(_call("tensor_reduce", out=rt_pad[:], in_=cmp3, axis=AX.X, op=ALU.add), r=["big"], w=["rt_pad"])
                dve(_call("tensor_scalar", out=rt_pad[:], in0=rt_pad[:], scalar1=float(BLK), scalar2=None, op0=ALU.mult),
                    r=["rt_pad"], w=["rt_pad"])
                dve(_call("tensor_tensor_scan", out=rt_pend[:], data0=ones_f[:, 0:NE], data1=rt_pad[:], initial=0.0,
                          op0=ALU.mult, op1=ALU.add), r=["rt_pad", "ones_f"], w=["rt_pend"])
                dve(_call("tensor_tensor", out=rt_pst[:], in0=rt_pend[:], in1=rt_pad[:], op=ALU.subtract),
                    r=["rt_pend", "rt_pad"], w=["rt_pst"])
                e4f = e4[:].rearrange("p t k -> p (t k)")
                destf = rank4[:].rearrange("p t k -> p (t k)")
                dtmp = big[:, 0:nt * 4]
                for ee in range(NE):
                    dve(_call("tensor_scalar", out=dtmp, in0=e4f, scalar1=float(ee), scalar2=rt_pst[:, ee:ee + 1],
                              op0=ALU.is_equal, op1=ALU.mult), r=["e4", "rt_pst"], w=["big"])
                    dve(_call("tensor_tensor", out=destf, in0=destf, in1=dtmp, op=ALU.add), r=["big", "rank4"], w=["rank4"])
                dve(_call("tensor_copy", out=dest_i[:].rearrange("p t k -> p (t k)"), in_=destf), r=["rank4"], w=["dest_i"])
                cmpb = big[:, 0:NB * NE].rearrange("p (b e) -> p b e", e=NE)
                dve(_call("tensor_tensor", out=cmpb, in0=bvals[:].unsqueeze(2).broadcast_to([128, NB, NE]),
                          in1=rt_pend[:].unsqueeze(1).broadcast_to([128, NB, NE]), op=ALU.is_ge),
                    r=["bvals", "rt_pend"], w=["big"])
                dve(_call("tensor_reduce", out=ebf[:], in_=cmpb, axis=AX.X, op=ALU.add), r=["big"], w=["ebf"])
                dve(_call("tensor_scalar", out=ebf[:], in0=ebf[:], scalar1=float(NE - 1), scalar2=float(l * NE),
                          op0=ALU.min, op1=ALU.add), r=["ebf"], w=["ebf"])
                dve(_call("tensor_copy", out=Id_i[:], in_=ebf[:]), r=["ebf"], w=["Id_i"])
                dve(_call("tensor_scalar", out=big[:, 0:NB], in0=ebf[:], scalar1=128.0, scalar2=pidx[:, 0:1],
                          op0=ALU.mult, op1=ALU.add), r=["ebf", "pidx"], w=["big"])
                dve(_call("tensor_copy", out=Ib_i[:], in_=big[:, 0:NB]), r=["big"], w=["Ib_i"])
                iw3 = big[:, 0:NB * 8].rearrange("p (b k) -> p b k", k=8)
                dve(_call("tensor_scalar", out=ebf[:], in0=ebf[:], scalar1=1024.0, scalar2=None, op0=ALU.mult),
                    r=["ebf", "Ib_i", "big"], w=["ebf"])
                dve(_call("tensor_tensor", out=iw3, in0=ebf[:].unsqueeze(2).broadcast_to([128, NB, 8]),
                          in1=pidx[:].unsqueeze(1).broadcast_to([128, NB, 8]), op=ALU.add), r=["ebf", "pidx"], w=["big"])
                dve(_call("tensor_copy", out=IW_i[:], in_=iw3), r=["big"], w=["IW_i"])
                dve(_call("memset", cum[:], 0.0), r=["rt_pad"], w=["cum"])
                S.phase_barrier()
                chk("route")

                for i in range(nt):
                    q = i % 2
                    dma_sp("hs%d" % q, hs[q], h2buf_d[i * 128:(i + 1) * 128, :], w=["hs%d" % q])
                    for k4 in range(4):
                        S.add("pool", _call("indirect_dma_start", out=hslots_d,
                                            out_offset=bass.IndirectOffsetOnAxis(ap=dest_i[:, i, k4:k4 + 1], axis=0),
                                            in_=hs[q], in_offset=None), ["hs%d" % q, "dest_i"], [], dma="sc%d" % q)
                S.phase_barrier()
                chk("scatter")

                ycount = 0
                gcount = 0
                def load_rows(bb):
                    hq = bb % 2
                    dma_sp("hr%d" % hq, hrows[hq], hslots_d[bb * BLK:(bb + 1) * BLK, :].rearrange("(t p) d -> p t d", p=128),
                           w=["hrows%d" % hq])

                load_rows(0)
                for b in range(NB):
                    hb = b % 2
                    if b + 1 < NB:
                        load_rows(b + 1)
                    for k in range(8):
                        S.add("pool", _call("indirect_dma_start", out=Wgu[hb][:, k, :], out_offset=None, in_=wgu2d,
                                            in_offset=bass.IndirectOffsetOnAxis(ap=IW_i[:, b, k:k + 1], axis=0)),
                              ["IW_i"], ["Wgu%d_%d" % (hb, k)], dma="wgu%d" % hb)
                    S.add("pool", _call("indirect_dma_start", out=bgub[hb][:], out_offset=None, in_=bgu_d,
                                        in_offset=bass.IndirectOffsetOnAxis(ap=Ib_i[:, b:b + 1], axis=0)),
                          ["Ib_i"], ["bgub%d" % hb], dma="bg%d" % hb)
                    S.add("pool", _call("indirect_dma_start", out=bdbc[hb][:], out_offset=None, in_=bdn_d,
                                        in_offset=bass.IndirectOffsetOnAxis(ap=Id_i[:, b:b + 1], axis=0)),
                          ["Id_i"], ["bdbc%d" % hb], dma="bd%d" % hb)
                    for j in range(8):
                        S.add("pool", _call("indirect_dma_start", out=Wd[hb][:, j, :], out_offset=None, in_=wdn2d,
                                            in_offset=bass.IndirectOffsetOnAxis(ap=IW_i[:, b, j:j + 1], axis=0)),
                              ["IW_i"], ["Wd%d_%d" % (hb, j)], dma="wd%d" % hb)
                    for k in range(8):
                        tb = 6 + k % 2
                        psb = ps[tb][:].bitcast(BF16)
                        pe(tr_group_b([(psb[:, tt * 128:(tt + 1) * 128], hrows[hb][:, tt, k * 128:(k + 1) * 128])
                                       for tt in range(4)]), r=["hrows%d" % hb, "ident_b"], w=["ps%d" % tb])
                        if k % 2 == 0:
                            dve(_call("tensor_copy", out=h2Tb[hb][:, k, :], in_=psb[:, 0:512]), r=["ps%d" % tb],
                                w=["h2Tb%d_%d" % (hb, k)])
                        else:
                            actop(_call("activation", out=h2Tb[hb][:, k, :], in_=psb[:, 0:512], func=AF.Identity),
                                  r=["ps%d" % tb], w=["h2Tb%d_%d" % (hb, k)])
                    dve(_call("tensor_scalar", out=bgs[hb][:], in0=bgub[hb][:, 0:8], scalar1=ALPHA, scalar2=None,
                              op0=ALU.mult), r=["bgub%d" % hb], w=["bgs%d" % hb])
                    dve(_call("tensor_scalar", out=bu1[hb][:], in0=bgub[hb][:, 8:16], scalar1=1.0, scalar2=None,
                              op0=ALU.add), r=["bgub%d" % hb], w=["bu1%d" % hb])
                    h2keys = ["h2Tb%d_%d" % (hb, k) for k in range(8)]
                    wgkeys = ["Wgu%d_%d" % (hb, k) for k in range(8)]
                    wdkeys = ["Wd%d_%d" % (hb, j) for j in range(8)]
                    for fj in range(8):
                        ab = act_sb[fj // 4]
                        j = fj % 4
                        gb = gcount % 2
                        gcount += 1
                        pe(mm_group([(ps[gb][:, :], Wgu[hb][:, k, fj * 128:(fj + 1) * 128], h2Tb[hb][:, k, :], k == 0, k == 7)
                                     for k in range(8)]), r=wgkeys + h2keys, w=["ps%d" % gb])
                        pe(mm_group([(ps[2 + gb][:, :], Wgu[hb][:, k, D + fj * 128: D + (fj + 1) * 128], h2Tb[hb][:, k, :],
                                      k == 0, k == 7) for k in range(8)]), r=wgkeys + h2keys, w=["ps%d" % (2 + gb)])
                        actop(_call("activation", out=sg_t[:], in_=ps[gb][:], func=AF.Sigmoid, scale=ALPHA,
                                    bias=bgs[hb][:, fj:fj + 1]), r=["ps%d" % gb, "bgs%d" % hb], w=["sg"])
                        dve(_call("tensor_scalar", out=gl_t[:], in0=ps[gb][:], scalar1=bgub[hb][:, fj:fj + 1], scalar2=LIMIT,
                                  op0=ALU.add, op1=ALU.min), r=["ps%d" % gb, "bgub%d" % hb], w=["gl"])
                        dve(_call("tensor_scalar", out=t1_t[:], in0=ps[2 + gb][:], scalar1=bu1[hb][:, fj:fj + 1],
                                  scalar2=1.0 - LIMIT, op0=ALU.add, op1=ALU.max), r=["ps%d" % (2 + gb), "bu1%d" % hb], w=["t1"])
                        dve(_call("scalar_tensor_tensor", out=aa_t[:], in0=sg_t[:], scalar=SIG_MAX, in1=gl_t[:],
                                  op0=ALU.min, op1=ALU.mult), r=["sg", "gl"], w=["aa"])
                        dve(_call("scalar_tensor_tensor", out=ab[:, j, :], in0=t1_t[:], scalar=1.0 + LIMIT, in1=aa_t[:],
                                  op0=ALU.min, op1=ALU.mult), r=["t1", "aa"], w=["act%d" % fj])
                    for tt in range(4):
                        for dh in range(2):
                            yb = 4 + ycount % 2
                            ycount += 1
                            specs = []
                            for fj in range(8):
                                specs.append((ps[yb][:, :], act_sb[fj // 4][:, fj % 4, tt * 128:(tt + 1) * 128],
                                              Wd[hb][:, fj, dh * 512:(dh + 1) * 512], fj == 0, fj == 7))
                            pe(mm_group(specs), r=["act%d" % fj for fj in range(8)] + wdkeys, w=["ps%d" % yb])
                            dve(_call("tensor_tensor", out=ystage[:, tt, dh * 512:(dh + 1) * 512], in0=ps[yb][:],
                                      in1=bdbc[hb][:, dh * 512:(dh + 1) * 512], op=ALU.add),
                                r=["ps%d" % yb, "bdbc%d" % hb], w=["ystage%d_%d" % (tt, dh)])
                    dma_sp("yst", yslots_d[b * BLK:(b + 1) * BLK, :].rearrange("(t p) d -> p t d", p=128), ystage,
                           r=["ystage%d_%d" % (tt, dh) for tt in range(4) for dh in range(2)])
                    if b == 0:
                        chk("blk0")
                S.phase_barrier()
                chk("blocks")

                WRf = WR[:, 0:16384].bitcast(F32)
                ykb = [yk, [WRf[:, k * 1024:(k + 1) * 1024] for k in range(4)]]
                ex1b = [ex1, WRf[:, 4096:5120]]
                eab = [ea, WRf[:, 5120:6144]]

                def comb_loads(ii):
                    q = ii % 2
                    dma_sp("ex1_%d" % q, ex1b[q], x1_d[ii * 128:(ii + 1) * 128, :], w=["ex1_%d" % q])
                    for k4 in range(4):
                        S.add("pool", _call("indirect_dma_start", out=ykb[q][k4], out_offset=None, in_=yslots_d,
                                            in_offset=bass.IndirectOffsetOnAxis(ap=dest_i[:, ii, k4:k4 + 1], axis=0)),
                              ["dest_i"], ["yk%d_%d" % (q, k4)], dma="yk%d_%d" % (q, k4))

                comb_loads(0)
                for i in range(nt):
                    r0 = i * 128
                    q = i % 2
                    if i + 1 < nt:
                        comb_loads(i + 1)
                    ykq, eaq, ex1q = ykb[q], eab[q], ex1b[q]
                    actop(_call("activation", out=eaq, in_=ykq[0], func=AF.Identity, scale=G4[:, i, 0:1]),
                          r=["yk%d_0" % q, "G4"], w=["ea%d" % q])
                    for k4 in range(1, 4):
                        dve(_call("scalar_tensor_tensor", out=eaq, in0=ykq[k4], scalar=G4[:, i, k4:k4 + 1], in1=eaq,
                                  op0=ALU.mult, op1=ALU.add), r=["yk%d_%d" % (q, k4), "G4", "ea%d" % q], w=["ea%d" % q])
                    S.add("pool", _call("tensor_tensor", out=eaq, in0=eaq, in1=G2bc[:], op=ALU.mult),
                          ["ea%d" % q, "G2bc"], ["ea%d" % q])
                    S.add("pool", _call("tensor_tensor", out=eaq, in0=eaq, in1=ex1q, op=ALU.add),
                          ["ea%d" % q, "ex1_%d" % q], ["ea%d" % q])
                    dma_sp("ost%d" % q, dst_d[r0:r0 + 128, :], eaq, r=["ea%d" % q])
                S.phase_barrier()
                chk("layer")
        except _Stop:
            S.phase_barrier()


        S.add("sp", lambda e: _Nop())
        S.emit(nc, st)
    return nc


def _col(v):
    v = np.asarray(v, np.float32)
    lead = int(np.prod(v.shape[:-1])) if v.ndim > 1 else 1
    n = v.shape[-1] // 128
    return np.ascontiguousarray(v.reshape(lead * n, 128).T)


def make_in_maps(inputs, ncores, ntok, depth):
    ident, mbc, btc = _const_tables()
    us_, io_, th_, bv_, pi_ = _route_tables(ntok)
    f = lambda k: np.ascontiguousarray(np.asarray(inputs[k], np.float32)[:depth])
    shared = {
        "ada_w": f("ada_w"),
        "adab_col": _col(f("ada_b")),
        "n1g_col": _col(f("norm1_g")),
        "n2g_col": _col(f("norm2_g")),
        "w_in": f("w_in"),
        "gqk": np.ascontiguousarray(np.concatenate([f("q_norm_g"), f("k_norm_g")], axis=1).reshape(1, depth * 128)),
        "sinks": np.ascontiguousarray(f("attn_sinks").reshape(1, depth * 8)),
        "pool_w": f("pool_w"),
        "pb_col": _col(f("pool_b")),
        "psc_col": _col(f("pool_scale")),
        "w_out": f("w_out"),
        "router_w": f("router_w"),
        "router_b": np.ascontiguousarray(f("router_b").reshape(1, depth * NE)),
        "w_gu": f("expert_w_gu"),
        "bgu_t": np.ascontiguousarray(f("expert_b_gu").reshape(depth, NE, 16, 128).transpose(0, 1, 3, 2).reshape(depth * NE * 128, 16)),
        "w_down": f("expert_w_down"),
        "bdn_t": np.ascontiguousarray(f("expert_b_down").reshape(depth * NE, D)),
        "cst_ident": ident,
        "cst_mb": np.ascontiguousarray(mbc.reshape(128, 1024)),
        "cst_bt": btc,
        "cst_ustrict": us_, "cst_iota": io_, "cst_thr": th_, "cst_bvals": bv_, "cst_pidx": pi_,
    }
    x = np.asarray(inputs["x"], np.float32)
    c = np.asarray(inputs["c"], np.float32)
    pos = np.asarray(inputs["positions"], np.int32)
    maps = []
    for b in range(ncores):
        m = dict(shared)
        m["x"] = np.ascontiguousarray(x[b, :ntok])
        m["ccol"] = np.ascontiguousarray(c[b].reshape(8, 128).T)
        m["poscol"] = np.ascontiguousarray(pos[b, :ntok].reshape(ntok // 128, 128).T)
        maps.append(m)
    return maps


_NC_CACHE = {}


def run(inputs, ncores, ntok, depth, trace=False, stop=None):
    key = (ntok, depth, stop)
    if key not in _NC_CACHE:
        _NC_CACHE[key] = build_program(ntok, depth, stop)
    nc = _NC_CACHE[key]
    maps = make_in_maps(inputs, ncores, ntok, depth)
    res = run_bass_kernel_spmd(nc, maps, core_ids=list(range(ncores)), trace=trace)
    out = np.stack([np.asarray(r["out"]) for r in res.results], axis=0)
    return out.astype(np.float32), res


def kernel(**inputs):
    out, _ = run(inputs, 8, 8192, 2)
    return out
```

```python
import math
from contextlib import ExitStack

import numpy as np
import concourse.bass as bass
import concourse.mybir as mybir
from concourse.bass_utils import run_bass_kernel_spmd

F32 = mybir.dt.float32
BF16 = mybir.dt.bfloat16
I32 = mybir.dt.int32
AF = mybir.ActivationFunctionType
ALU = mybir.AluOpType
AX = mybir.AxisListType

D = 1024
NE = 32
HD = 64
EPS = 1e-6
LIMIT = 7.0
ALPHA = 1.702
THETA = 500000.0
TG = 1024
TPG = TG // 128
BLK = 512
SIG_MAX = float(1.0 / (1.0 + math.exp(-ALPHA * LIMIT)))
TWO_PI = 2.0 * math.pi
PI_HI = 6.28125
PI_LO = TWO_PI - PI_HI


class _Op:
    __slots__ = ("eng", "fn", "deps", "dmawait", "semkey", "sem", "val", "signal")


class Sched:
    ENGS = ("pe", "act", "dve", "pool", "sp")
    ROT = 20000

    def __init__(self):
        self.ops = {e: [] for e in self.ENGS}
        self.buf = {}
        self.barrier = set()
        self.barrier_dma = {}
        self.dma_issued = {}
        self.dma_last = {}

    def add(self, eng, fn, reads=(), writes=(), dma=None, extra=()):
        o = _Op()
        o.eng = eng
        o.fn = fn
        o.semkey = dma
        o.sem = None
        o.val = 0
        o.signal = dma is not None
        deps = set(self.barrier)
        deps.update(extra)
        writes = list(writes) + [k for k in reads if k.startswith("ps") and k not in writes]
        for k in reads:
            st = self.buf.get(k)
            if st is not None and st[0] is not None:
                deps.add(st[0])
        for k in writes:
            st = self.buf.get(k)
            if st is not None:
                if st[0] is not None and not st[1]:
                    deps.add(st[0])
                deps.update(st[1])
        o.deps = set()
        o.dmawait = dict(self.barrier_dma)
        for d in deps:
            if d.semkey is not None:
                o.dmawait[d.semkey] = self.dma_issued[d.semkey]
            else:
                o.deps.add(d)
        wset = set(writes)
        for k in reads:
            if k in wset:
                continue
            st = self.buf.get(k)
            if st is None:
                st = self.buf[k] = [None, []]
            st[1].append(o)
        for k in writes:
            self.buf[k] = [o, []]
        self.ops[eng].append(o)
        if dma is not None:
            self.dma_issued[dma] = self.dma_issued.get(dma, 0) + 16
            o.val = self.dma_issued[dma]
            self.dma_last[dma] = o
        return o

    def phase_barrier(self):
        b = set()
        for e in ("pe", "act", "dve", "pool"):
            for o in reversed(self.ops[e]):
                if o.semkey is None:
                    b.add(o)
                    break
        self.barrier_dma = dict(self.dma_issued)
        self.barrier = b

    def emit(self, nc, stack):
        for e in self.ENGS:
            for o in self.ops[e]:
                for d in o.deps:
                    if d.eng == "pe" and o.eng == "pe":
                        continue
                    d.signal = True
        for e in self.ENGS:
            cur = None
            cnt = 0
            n = 0
            for o in self.ops[e]:
                if o.semkey is not None or not o.signal:
                    continue
                if cur is None or cnt >= self.ROT:
                    cur = stack.enter_context(nc.semaphore("s_%s_%d" % (e, n)))
                    n += 1
                    cnt = 0
                cnt += 1
                o.sem = cur
                o.val = cnt
        dsem = {}
        for k in self.dma_issued:
            dsem[k] = stack.enter_context(nc.semaphore("d_%s" % k))
        block = stack.enter_context(nc.Block())

        def run(engname, eng):
            waited = {}
            for o in self.ops[engname]:
                need = {}
                for d in o.deps:
                    if d.eng == "pe" and engname == "pe":
                        continue
                    key = id(d.sem)
                    if key not in need or need[key][1] < d.val:
                        need[key] = (d.sem, d.val)
                for k, v in o.dmawait.items():
                    key = id(dsem[k])
                    if key not in need or need[key][1] < v:
                        need[key] = (dsem[k], v)
                for key, (sem, v) in need.items():
                    if waited.get(key, 0) >= v:
                        continue
                    eng.wait_ge(sem, v)
                    waited[key] = v
                ins = o.fn(eng)
                if o.semkey is not None:
                    ins.then_inc(dsem[o.semkey], 16)
                elif o.signal:
                    ins.then_inc(o.sem, 1)

        @block.tensor
        def _(eng):
            run("pe", eng)

        @block.scalar
        def _(eng):
            run("act", eng)

        @block.vector
        def _(eng):
            run("dve", eng)

        @block.gpsimd
        def _(eng):
            run("pool", eng)

        @block.sync
        def _(eng):
            run("sp", eng)


def _call(name, *args, **kw):
    def fn(e):
        return getattr(e, name)(*args, **kw)
    return fn


class _Nop:
    def then_inc(self, *a):
        return self


def _const_tables():
    ident = np.eye(128, dtype=np.float32)
    kk = np.arange(128)[:, None]
    qq = np.arange(128)[None, :]
    NEG = -30000.0
    mb_cur = np.where(qq >= kk, 0.0, NEG).astype(np.float32)
    mb_prev = np.where(kk > qq, 0.0, NEG).astype(np.float32)
    mb = np.stack([np.tile(mb_prev, (1, 4)), np.tile(mb_cur, (1, 4))], axis=1)
    bt = np.zeros((128, 3, 4, 128), np.float32)
    for g, w in enumerate((2, 4, 8, 16)):
        for t in range(128):
            for j in range(t - w + 1, t + 1):
                if j >= 0:
                    bt[j, 0, g, t] += 1.0 / w
                    bt[j, 2, g, t] += 1.0 / min(t + 1, w)
                else:
                    bt[j + 128, 1, g, t] += 1.0 / w
            bt[t, 0, g, t] -= 1.0
            bt[t, 2, g, t] -= 1.0
    return ident, mb.astype(np.float32), bt.reshape(128, 12 * 128)


def _route_tables(ntok):
    n_slots = ntok * 4 + NE * BLK
    nb = n_slots // BLK
    p = np.arange(128, dtype=np.float32)[:, None]
    ustrict = (np.arange(128)[:, None] < np.arange(128)[None, :]).astype(np.float32)
    iota = np.tile(np.arange(NE, dtype=np.float32)[None, :], (128, 1))
    thr = np.tile((np.arange(16, dtype=np.float32) * BLK)[None, :], (128, 1))
    bvals = np.tile((np.arange(nb, dtype=np.float32) * BLK)[None, :], (128, 1))
    pidx = (np.arange(8, dtype=np.float32)[None, :] * 128 + p).astype(np.float32)
    return ustrict, iota, thr, bvals, pidx


def _inv_freq():
    e = -np.arange(0, 16, 2, dtype=np.float32) / np.float32(16)
    return [float(v) for v in np.power(np.float32(THETA), e).astype(np.float32)]


class _Stop(Exception):
    pass


def build_program(ntok, depth, stop=None):
    nt = ntok // 128
    ng = ntok // TG
    n_slots = ntok * 4 + NE * BLK
    NB = n_slots // BLK
    nc = bass.Bass("TRN2", target_bir_lowering=False)
    S = Sched()
    invf = _inv_freq()

    def din(name, shape, dt=F32):
        return nc.dram_tensor(name, list(shape), dt, kind="ExternalInput").ap()

    x_d = din("x", [ntok, D])
    ccol_d = din("ccol", [128, 8])
    pos_d = din("poscol", [128, nt], I32)
    adaw_d = din("ada_w", [depth, D, 6 * D])
    adab_d = din("adab_col", [128, depth * 48])
    n1g_d = din("n1g_col", [128, depth * 8])
    n2g_d = din("n2g_col", [128, depth * 8])
    win_d = din("w_in", [depth, D, 1280])
    gqk_d = din("gqk", [1, depth * 128])
    sink_d = din("sinks", [1, depth * 8])
    poolw_d = din("pool_w", [depth, 4, 128, 128])
    pb_d = din("pb_col", [128, depth * 4])
    psc_d = din("psc_col", [128, depth * 4])
    wout_d = din("w_out", [depth, D, D])
    rw_d = din("router_w", [depth, D, NE])
    rb_d = din("router_b", [1, depth * NE])
    wgu_d = din("w_gu", [depth, NE, D, 2 * D])
    bgu_d = din("bgu_t", [depth * NE * 128, 16])
    wdn_d = din("w_down", [depth, NE, D, D])
    bdn_d = din("bdn_t", [depth * NE, D])
    cid_d = din("cst_ident", [128, 128])
    cmb_d = din("cst_mb", [128, 2 * 512])
    cbt_d = din("cst_bt", [128, 12 * 128])
    cus_d = din("cst_ustrict", [128, 128])
    cio_d = din("cst_iota", [128, NE])
    cth_d = din("cst_thr", [128, 16])
    cbv_d = din("cst_bvals", [128, NB])
    cpi_d = din("cst_pidx", [128, 8])
    out_d = nc.dram_tensor("out", [ntok, D], F32, kind="ExternalOutput").ap()
    x1_d = nc.dram_tensor("x1buf", [ntok, D], F32, kind="Internal").ap()
    xr_d = nc.dram_tensor("xres", [ntok, D], F32, kind="Internal").ap()
    h2buf_d = nc.dram_tensor("h2buf", [ntok, D], BF16, kind="Internal").ap()
    hslots_d = nc.dram_tensor("hslots", [n_slots, D], BF16, kind="Internal").ap()
    yslots_d = nc.dram_tensor("yslots", [n_slots, D], F32, kind="Internal").ap()
    wgu2d = wgu_d.rearrange("l e d f -> (l e d) f")
    wdn2d = wdn_d.rearrange("l e d f -> (l e d) f")

    with ExitStack() as st:
        def sb(name, shape, dt=F32):
            return st.enter_context(nc.sbuf_tensor("sb_" + name, list(shape), dt))

        AR = sb("AR", [128, 8192], F32)
        WR = sb("WR", [128, 49152], BF16)
        LR = sb("LR", [128, 4096], F32)
        ident_f = sb("ident_f", [128, 128])
        ident_b = sb("ident_b", [128, 128], BF16)
        ones_f = sb("ones_f", [128, 128])
        mb = sb("mb", [128, 2, 512], BF16)
        bt = sb("bt", [128, 12, 128])
        wpool = sb("wpool", [128, depth * 4, 128])
        pb_col = sb("pb_col", [128, depth * 4])
        psc_col = sb("psc_col", [128, depth * 4])
        rw = sb("rw", [128, depth * 8, NE])
        rb_bc = sb("rb_bc", [128, depth * NE])
        gqk_bc = sb("gqk_bc", [128, depth * 128])
        esink = sb("esink", [128, depth * 8])
        cos_t = sb("cos_t", [128, nt, 8])
        sin_t = sb("sin_t", [128, nt, 8])
        ccol = sb("ccol", [128, 8])
        cact = sb("cact", [128, 8])
        adab = sb("adab", [128, depth * 48])
        n1g = sb("n1g", [128, depth * 8])
        n2g = sb("n2g", [128, depth * 8])
        modT = sb("modT", [128, depth * 48])
        A1 = sb("A1", [128, depth * 8])
        A2 = sb("A2", [128, depth * 8])
        G1bc = LR[:, 0:1024]
        G2bc = sb("G2bc", [128, D])
        gtmp = sb("gtmp", [128, 128])
        A2bc = LR[:, 1024:2048]
        S2bc = LR[:, 2048:3072]
        ustrict = sb("ustrict", [128, 128])
        iotaE = sb("iotaE", [128, NE])
        thr512 = sb("thr512", [128, 16])
        bvals = sb("bvals", [128, NB])
        pidx = sb("pidx", [128, 8])
        cum = sb("cum", [128, NE])
        rt_pad = sb("rt_pad", [128, NE])
        rt_pend = sb("rt_pend", [128, NE])
        rt_pst = sb("rt_pst", [128, NE])
        ebf = sb("ebf", [128, NB])
        rank4 = sb("rank4", [128, nt, 4])
        e4 = sb("e4", [128, nt, 4])
        G4 = sb("G4", [128, nt, 4])
        dest_i = sb("dest_i", [128, nt, 4], I32)
        IW_i = sb("IW_i", [128, NB, 8], I32)
        Ib_i = sb("Ib_i", [128, NB], I32)
        Id_i = sb("Id_i", [128, NB], I32)
        bgub = [sb("bgub%d" % i, [128, 16]) for i in range(2)]
        bgs = [sb("bgs%d" % i, [128, 8]) for i in range(2)]
        bu1 = [sb("bu1%d" % i, [128, 8]) for i in range(2)]
        bdbc = [sb("bdbc%d" % i, [128, D], BF16) for i in range(2)]
        kT = [sb("kT%d" % i, [128, 128], BF16) for i in range(2)]
        vaug = [sb("vaug%d" % i, [128, 2, 65], BF16) for i in range(2)]
        usb = [LR[:, 3072 + i * 512: 3072 + (i + 1) * 512] for i in range(2)]
        h2Tb = [sb("h2Tb%d" % i, [128, 8, BLK], BF16) for i in range(2)]
        act_sb = [LR[:, 2048 + i * 1024: 2048 + (i + 1) * 1024].bitcast(BF16).rearrange("p (j t) -> p j t", j=4)
                  for i in range(2)]
        sg_t = LR[:, 0:512]
        gl_t = LR[:, 512:1024]
        t1_t = LR[:, 1024:1536]
        aa_t = LR[:, 1536:2048]
        sm = sb("sm", [128, 256])
        posf = sb("posf", [128, nt])
        cst = sb("cst", [128, 8])
        ps = [st.enter_context(nc.psum_tensor("ps%d" % i, [128, 512], F32)) for i in range(8)]

        def arv(off, n):
            return AR[:, off:off + n]

        xt2 = [arv(0, 1024), WR[:, 25088:27136].bitcast(F32)]
        xn = arv(1024, 1024)
        x1 = arv(2048, 1024)
        xn2 = arv(3072, 1024)
        h2Tf = arv(4096, 1024).rearrange("p (k t) -> p k t", k=8)
        qn = arv(5120, 640)
        attn = arv(5760, 512)
        pooledT = arv(6272, 512)
        sq = arv(6784, 640)
        rtmp = arv(7424, 320).rearrange("p (a h d) -> p a h d", a=4, h=10)
        lg = arv(7744, 32)
        msk = arv(7776, 32)
        exv = arv(7808, 32)
        exm = arv(7840, 32)
        sel4 = arv(7872, 128).rearrange("p (k e) -> p k e", k=4)
        prod4 = arv(8000, 128).rearrange("p (k e) -> p k e", k=4)
        trig = AR[:, 0:4 * nt * 8].rearrange("p (a n) -> p a n", a=4)
        trig_i = AR[:, 4096:4096 + nt * 8].bitcast(I32)
        big = AR[:, 0:max(NB * NE, NE * 16, nt * 4, NB * 8)]
        ystage = AR[:, 0:4096].rearrange("p (t d) -> p t d", t=4)
        yk = [arv(k * 1024, 1024) for k in range(4)]
        ex1 = arv(4096, 1024)
        ea = arv(5120, 1024)
        adw = [arv(0, 4096).rearrange("p (k f) -> p k f", k=8),
               arv(4096, 4096).rearrange("p (k f) -> p k f", k=8)]

        w_in = WR[:, 0:10240].rearrange("p (k f) -> p k f", k=8)
        w_out = WR[:, 10240:18432].rearrange("p (k f) -> p k f", k=8)
        junk = WR[:, 18432:19456]
        hT = WR[:, 19456:20480].rearrange("p (k t) -> p k t", k=8)
        qT = WR[:, 20480:20992]
        PT = [[WR[:, 20992 + (b * 2 + h) * 512: 20992 + (b * 2 + h + 1) * 512] for h in range(2)]
              for b in range(2)]
        mixT = WR[:, 23040:24064].rearrange("p (k t) -> p k t", k=8)
        h2row = WR[:, 24064:25088]
        Wgu = [WR[:, p * 16384:(p + 1) * 16384].rearrange("p (k f) -> p k f", k=8) for p in range(2)]
        Wd = [WR[:, 32768 + q * 8192: 32768 + (q + 1) * 8192].rearrange("p (j d) -> p j d", j=8) for q in range(2)]
        hs = [WR[:, q * 1024:(q + 1) * 1024] for q in range(2)]
        hrows = [arv(4096 + q * 2048, 2048).bitcast(BF16).rearrange("p (t d) -> p t d", t=4) for q in range(2)]

        def smv(i, n=1):
            return sm[:, i:i + n]

        def dve(fn, r=(), w=()):
            return S.add("dve", fn, r, w)

        def actop(fn, r=(), w=()):
            return S.add("act", fn, r, w)

        def pe(fn, r=(), w=()):
            return S.add("pe", fn, r, w)

        def dma_sp(key, out, in_, r=(), w=()):
            return S.add("sp", _call("dma_start", out=out, in_=in_), r, w, dma=key)

        def dma_pool(key, out, in_, r=(), w=()):
            return S.add("pool", _call("dma_start", out=out, in_=in_), r, w, dma=key)

        def mm_group(specs):
            def fn(e):
                ins = None
                for (o, l, r_, s0, s1) in specs:
                    ins = e.matmul(o, l, r_, start=s0, stop=s1)
                return ins
            return fn

        def tr_group_b(specs):
            def fn(e):
                ins = None
                for (o, i_) in specs:
                    ins = e.transpose(out=o, in_=i_, identity=ident_b[:])
                return ins
            return fn

        def tr_group(specs):
            def fn(e):
                ins = None
                for (o, i_) in specs:
                    ins = e.transpose(out=o, in_=i_, identity=ident_f[:])
                return ins
            return fn

        dma_sp("c0", ident_f[:], cid_d, w=["ident_f"])
        dma_sp("c0", bt[:], cbt_d.rearrange("p (a t) -> p a t", a=12), w=["bt"])
        dma_sp("c0", wpool[:], poolw_d.rearrange("l g c d -> c (l g) d"), w=["wpool"])
        dma_sp("c0", pb_col[:], pb_d, w=["pb_col"])
        dma_sp("c0", psc_col[:], psc_d, w=["psc_col"])
        dma_sp("c0", rw[:], rw_d.rearrange("l (k p) e -> p (l k) e", p=128), w=["rw"])
        dma_sp("c0", rb_bc[:], rb_d[0:1, :].broadcast_to([128, depth * NE]), w=["rb_bc"])
        dma_sp("c0", gqk_bc[:], gqk_d[0:1, :].broadcast_to([128, depth * 128]), w=["gqk_bc"])
        dma_sp("c0", esink[:], sink_d[0:1, :].broadcast_to([128, depth * 8]), w=["esink"])
        dma_sp("c0", ccol[:], ccol_d, w=["ccol"])
        dma_sp("c0", adab[:], adab_d, w=["adab"])
        dma_sp("c0", n1g[:], n1g_d, w=["n1g"])
        dma_sp("c0", n2g[:], n2g_d, w=["n2g"])
        dma_sp("c0", ustrict[:], cus_d, w=["ustrict"])
        dma_sp("c0", iotaE[:], cio_d, w=["iotaE"])
        dma_sp("c0", thr512[:], cth_d, w=["thr512"])
        dma_sp("c0", bvals[:], cbv_d, w=["bvals"])
        dma_sp("c0", pidx[:], cpi_d, w=["pidx"])
        dma_sp("c0", trig_i[:, 0:nt], pos_d, w=["trig_i"])
        dma_pool("c1", mb[:], cmb_d.rearrange("p (a t) -> p a t", a=2), w=["mb"])

        dve(_call("memset", ones_f[:], 1.0), w=["ones_f"])
        dve(_call("memset", cst[:, 0:1], EPS), w=["cst0"])
        dve(_call("memset", cst[:, 1:2], math.pi / 2.0), w=["cst1"])
        for i in range(2):
            dve(_call("memset", vaug[i][:], 1.0), w=["vaug%d" % i])
            dve(_call("memset", usb[i][:], 0.0), w=["usb%d" % i])
        dve(_call("tensor_copy", out=ident_b[:], in_=ident_f[:]), r=["ident_f"], w=["ident_b"])
        actop(_call("activation", out=esink[:], in_=esink[:], func=AF.Exp), r=["esink"], w=["esink"])
        dve(_call("memset", cum[:], 0.0), w=["cum"])
        dve(_call("memset", WR[:, 0:8192], 0.0), w=["zsrc"])
        zsrc = WR[:, 0:8192].rearrange("p (a d) -> p a d", d=D)
        for z0 in range(0, n_slots, 1024):
            dma_pool("zf", hslots_d[z0:z0 + 1024, :].rearrange("(p a) d -> p a d", p=128), zsrc, r=["zsrc"])

        dve(_call("tensor_copy", out=posf[:], in_=trig_i[:, 0:nt]), r=["trig_i"], w=["posf"])
        ang = trig[:, 0, :]
        kf = trig[:, 1, :]
        rr = trig[:, 2, :]
        t3 = trig[:, 3, :]
        ang3 = ang.rearrange("p (t j) -> p t j", j=8)
        for j in range(8):
            dve(_call("tensor_scalar", out=ang3[:, :, j], in0=posf[:], scalar1=invf[j], scalar2=None,
                                               op0=ALU.mult), r=["posf"], w=["ang"])
        dve(_call("tensor_scalar", out=kf, in0=ang, scalar1=1.0 / TWO_PI, scalar2=0.5, op0=ALU.mult,
                                      op1=ALU.add), r=["ang"], w=["kf"])
        dve(_call("tensor_copy", out=trig_i[:], in_=kf), r=["kf"], w=["trig_i"])
        dve(_call("tensor_copy", out=kf, in_=trig_i[:]), r=["trig_i"], w=["kf"])
        dve(_call("scalar_tensor_tensor", out=rr, in0=kf, scalar=-PI_HI, in1=ang, op0=ALU.mult, op1=ALU.add),
            r=["kf", "ang"], w=["rr"])
        dve(_call("scalar_tensor_tensor", out=rr, in0=kf, scalar=-PI_LO, in1=rr, op0=ALU.mult, op1=ALU.add),
            r=["kf", "rr"], w=["rr"])
        dve(_call("tensor_scalar", out=t3, in0=rr, scalar1=math.pi, scalar2=-TWO_PI, op0=ALU.is_gt,
                                      op1=ALU.mult), r=["rr"], w=["t3"])
        dve(_call("tensor_tensor", out=rr, in0=rr, in1=t3, op=ALU.add), r=["rr", "t3"], w=["rr"])
        dve(_call("tensor_scalar", out=t3, in0=rr, scalar1=-math.pi, scalar2=TWO_PI, op0=ALU.is_lt,
                                      op1=ALU.mult), r=["rr"], w=["t3"])
        dve(_call("tensor_tensor", out=rr, in0=rr, in1=t3, op=ALU.add), r=["rr", "t3"], w=["rr"])
        dve(_call("tensor_scalar", out=rr, in0=rr, scalar1=math.pi, scalar2=-math.pi, op0=ALU.min,
                                      op1=ALU.max), r=["rr"], w=["rr"])
        dve(_call("scalar_tensor_tensor", out=t3, in0=rr, scalar=-1.0, in1=rr, op0=ALU.mult, op1=ALU.max), r=["rr"], w=["t3"])
        actop(_call("activation", out=sin_t[:].rearrange("p t j -> p (t j)"), in_=rr, func=AF.Sin),
              r=["rr"], w=["sin_t"])
        actop(_call("activation", out=cos_t[:].rearrange("p t j -> p (t j)"), in_=t3, func=AF.Sin,
                                     scale=-1.0, bias=cst[:, 1:2]), r=["t3", "cst1"], w=["cos_t"])

        S.phase_barrier()
        actop(_call("activation", out=cact[:], in_=ccol[:], func=AF.Silu), r=["ccol"], w=["cact"])
        npiece = 0
        for l in range(depth):
            for n in range(12):
                par = npiece % 2
                npiece += 1
                src = adaw_d[l].rearrange("(k p) f -> p k f", p=128)[:, :, n * 512:(n + 1) * 512]
                dma_sp("adw%d" % par, adw[par], src, w=["adw%d" % par])
                specs = []
                for jj in range(4):
                    col = l * 48 + n * 4 + jj
                    for k in range(8):
                        specs.append((ps[0][:, col:col + 1], adw[par][:, k, jj * 128:(jj + 1) * 128],
                                      cact[:, k:k + 1], k == 0, k == 7))
                pe(mm_group(specs), r=["adw%d" % par, "cact"], w=["ps0"])
        dve(_call("tensor_tensor", out=modT[:], in0=ps[0][:, 0:depth * 48], in1=adab[:], op=ALU.add),
            r=["ps0", "adab"], w=["modT"])
        modT3 = modT[:].rearrange("p (l j) -> p l j", j=48)
        dve(_call("scalar_tensor_tensor", out=A1[:].rearrange("p (l k) -> p l k", k=8), in0=modT3[:, :, 8:16],
                                             scalar=1.0, in1=n1g[:].rearrange("p (l k) -> p l k", k=8),
                                             op0=ALU.add, op1=ALU.mult), r=["modT", "n1g"], w=["A1"])
        dve(_call("scalar_tensor_tensor", out=A2[:].rearrange("p (l k) -> p l k", k=8), in0=modT3[:, :, 32:40],
                                             scalar=1.0, in1=n2g[:].rearrange("p (l k) -> p l k", k=8),
                                             op0=ALU.add, op1=ALU.mult), r=["modT", "n2g"], w=["A2"])

        def S1c(l, k):
            return modT[:, l * 48 + k: l * 48 + k + 1]

        def S2c(l, k):
            return modT[:, l * 48 + 24 + k: l * 48 + 24 + k + 1]

        S.phase_barrier()
        if stop == "setup":
            depth_run = 0
        else:
            depth_run = depth

        wslot = [0]
        def chk(name):
            if stop == name:
                raise _Stop()

        try:
            for l in range(depth_run):
                src_d = x_d if l == 0 else xr_d
                dst_d = out_d if l == depth - 1 else xr_d
                for (gt_tile, off, nm) in ((G1bc, 16, "G1bc"), (G2bc, 40, "G2bc"), (S2bc, 24, "S2bc"), (A2bc, -1, "A2bc")):
                    for half in range(2):
                        for kk in range(4):
                            k = half * 4 + kk
                            colap = (A2[:, l * 8 + k: l * 8 + k + 1] if off < 0 else
                                     modT[:, l * 48 + off + k: l * 48 + off + k + 1])
                            dve(_call("tensor_scalar",
                                out=gtmp[:], in0=ones_f[:], scalar1=colap,
                                scalar2=None, op0=ALU.mult), r=["ones_f", "modT", "A2"], w=["gtmp"])
                            pe(mm_group([(ps[1][:, kk * 128:(kk + 1) * 128], gtmp[:], ident_f[:], True, True)]),
                               r=["gtmp", "ident_f"], w=["ps1"])
                        dve(_call("tensor_copy",
                            out=gt_tile[:, half * 512:(half + 1) * 512], in_=ps[1][:]), r=["ps1"], w=[nm])
                S.phase_barrier()
                chk("gates")

                for g in range(ng):
                    wsrc = win_d[l].rearrange("(k p) f -> p k f", p=128)
                    for (c0, c1) in ((0, 512), (512, 1024), (1024, 1280)):
                        if g == 0:
                            dma_pool("win", w_in[:, :, c0:c1], wsrc[:, :, c0:c1], w=["w_in%d" % c0])
                    wsrc2 = wout_d[l].rearrange("(k p) f -> p k f", p=128)
                    for (c0, c1) in ((0, 512), (512, 1024)):
                        if g == 0:
                            dma_pool("wout", w_out[:, :, c0:c1], wsrc2[:, :, c0:c1], w=["w_out%d" % c0])

                    for tl in range(TPG):
                        i = g * TPG + tl
                        par = i % 2
                        r0 = i * 128
                        if i == 0:
                            dma_sp("xt0", xt2[0], src_d[0:128, :], w=["xt0"])
                        if i + 1 < nt:
                            dma_sp("xt%d" % (1 - par), xt2[1 - par], src_d[r0 + 128:r0 + 256, :], w=["xt%d" % (1 - par)])
                        xt = xt2[par]
                        xtk = "xt%d" % par
                        actop(_call("activation", out=junk, in_=xt, func=AF.Square, accum_out=smv(0)),
                              r=[xtk], w=["junk", "ss1"])
                        actop(_call("activation", out=smv(1), in_=smv(0), func=AF.Sqrt, scale=1.0 / D,
                                                     bias=cst[:, 0:1]), r=["ss1", "cst0"], w=["sd1"])
                        dve(_call("reciprocal", out=smv(2), in_=smv(1)), r=["sd1"], w=["rstd1"])
                        actop(_call("activation", out=xn, in_=xt, func=AF.Identity, scale=smv(2)),
                              r=[xtk, "rstd1"], w=["xn"])
                        for b in range(2):
                            pe(tr_group([(ps[b][:, j * 128:(j + 1) * 128], xn[:, (b * 4 + j) * 128:(b * 4 + j + 1) * 128])
                                         for j in range(4)]), r=["xn", "ident_f"], w=["ps%d" % b])
                            for j in range(4):
                                k = b * 4 + j
                                if j % 2 == 0:
                                    dve(_call("tensor_scalar",
                                        out=hT[:, k, :], in0=ps[b][:, j * 128:(j + 1) * 128],
                                        scalar1=A1[:, l * 8 + k: l * 8 + k + 1], scalar2=S1c(l, k),
                                        op0=ALU.mult, op1=ALU.add), r=["ps%d" % b, "A1", "modT"], w=["hT%d" % k])
                                else:
                                    actop(_call("activation",
                                        out=hT[:, k, :], in_=ps[b][:, j * 128:(j + 1) * 128], func=AF.Identity,
                                        scale=A1[:, l * 8 + k: l * 8 + k + 1], bias=S1c(l, k)),
                                        r=["ps%d" % b, "A1", "modT"], w=["hT%d" % k])
                        specs = []
                        for k in range(8):
                            specs.append((ps[2][:, :], hT[:, k, :], w_in[:, k, 0:512], k == 0, k == 7))
                            specs.append((ps[3][:, 0:256], hT[:, k, :], w_in[:, k, 512:768], k == 0, k == 7))
                            specs.append((ps[4][:, :], hT[:, k, :], w_in[:, k, 768:1280], k == 0, k == 7))
                        pe(mm_group(specs), r=["hT%d" % k for k in range(8)] + ["w_in0", "w_in512", "w_in1024"], w=["ps2", "ps3", "ps4"])
                        dve(_call("tensor_copy",
                            out=vaug[par][:, :, 0:64], in_=ps[3][:, 128:256].rearrange("p (h d) -> p h d", h=2)),
                            r=["ps3"], w=["vaug%d" % par])
                        actop(_call("activation", out=usb[par][:], in_=ps[4][:], func=AF.Identity),
                              r=["ps4"], w=["usb%d" % par])
                        actop(_call("activation", out=sq[:, 0:512], in_=ps[2][:], func=AF.Square), r=["ps2"], w=["sqq"])
                        actop(_call("activation", out=sq[:, 512:640], in_=ps[3][:, 0:128], func=AF.Square),
                              r=["ps3"], w=["sqk"])
                        dve(_call("tensor_reduce", out=smv(8, 10), in_=sq.rearrange("p (h d) -> p h d", d=64),
                                                      axis=AX.X, op=ALU.add), r=["sqq", "sqk"], w=["ssq"])
                        actop(_call("activation", out=smv(20, 10), in_=smv(8, 10), func=AF.Sqrt, scale=1.0 / HD,
                                                     bias=cst[:, 0:1]), r=["ssq", "cst0"], w=["sdq"])
                        dve(_call("reciprocal", out=smv(32, 10), in_=smv(20, 10)), r=["sdq"], w=["rq"])
                        qn3 = qn.rearrange("p (h d) -> p h d", d=64)
                        dve(_call("tensor_tensor",
                            out=qn[:, 0:512].rearrange("p (g hk d) -> p hk g d", g=4, hk=2),
                            in0=ps[2][:].rearrange("p (hk g d) -> p hk g d", hk=2, g=4),
                            in1=smv(32, 8).rearrange("p (hk g) -> p hk g", hk=2).unsqueeze(3).broadcast_to([128, 2, 4, 64]),
                            op=ALU.mult), r=["ps2", "rq"], w=["qnq"])
                        dve(_call("tensor_tensor",
                            out=qn3[:, 8:10, :], in0=ps[3][:, 0:128].rearrange("p (h d) -> p h d", d=64),
                            in1=smv(40, 2).unsqueeze(2).broadcast_to([128, 2, 64]), op=ALU.mult),
                            r=["ps3", "rq"], w=["qnk"])
                        dve(_call("tensor_tensor",
                            out=qn3[:, 0:8, :], in0=qn3[:, 0:8, :],
                            in1=gqk_bc[:, l * 128: l * 128 + 64].unsqueeze(1).broadcast_to([128, 8, 64]), op=ALU.mult),
                            r=["qnq", "gqk_bc"], w=["qnq"])
                        dve(_call("tensor_tensor",
                            out=qn3[:, 8:10, :], in0=qn3[:, 8:10, :],
                            in1=gqk_bc[:, l * 128 + 64: l * 128 + 128].unsqueeze(1).broadcast_to([128, 2, 64]),
                            op=ALU.mult), r=["qnk", "gqk_bc"], w=["qnk"])
                        cosb = cos_t[:, i, :].unsqueeze(1).broadcast_to([128, 10, 8])
                        sinb = sin_t[:, i, :].unsqueeze(1).broadcast_to([128, 10, 8])
                        t1v = qn3[:, :, 0:8]
                        t2v = qn3[:, :, 8:16]
                        dve(_call("tensor_tensor", out=rtmp[:, 0], in0=t1v, in1=cosb, op=ALU.mult),
                            r=["qnq", "qnk", "cos_t"], w=["rt0"])
                        dve(_call("tensor_tensor", out=rtmp[:, 1], in0=t2v, in1=sinb, op=ALU.mult),
                            r=["qnq", "qnk", "sin_t"], w=["rt1"])
                        dve(_call("tensor_tensor", out=rtmp[:, 2], in0=t2v, in1=cosb, op=ALU.mult),
                            r=["qnq", "qnk", "cos_t"], w=["rt2"])
                        dve(_call("tensor_tensor", out=rtmp[:, 3], in0=t1v, in1=sinb, op=ALU.mult),
                            r=["qnq", "qnk", "sin_t"], w=["rt3"])
                        dve(_call("tensor_tensor", out=t1v, in0=rtmp[:, 0], in1=rtmp[:, 1], op=ALU.subtract),
                            r=["rt0", "rt1"], w=["qnq", "qnk"])
                        dve(_call("tensor_tensor", out=t2v, in0=rtmp[:, 2], in1=rtmp[:, 3], op=ALU.add),
                            r=["rt2", "rt3"], w=["qnq", "qnk"])
                        pe(tr_group([(ps[5][:, gg * 128:(gg + 1) * 128], qn[:, gg * 128:(gg + 1) * 128]) for gg in range(4)]),
                           r=["qnq", "qnk", "ident_f"], w=["ps5"])
                        pe(tr_group([(ps[6][:, 0:128], qn[:, 512:640])]), r=["qnq", "qnk", "ident_f"], w=["ps6"])
                        dve(_call("tensor_copy", out=qT, in_=ps[5][:]), r=["ps5"], w=["qT"])
                        actop(_call("activation", out=kT[par][:], in_=ps[6][:, 0:128], func=AF.Identity),
                              r=["ps6"], w=["kT%d" % par])
                        blocks = ([0] if i > 0 else []) + [1]
                        sbank = {(0, 0): 7, (0, 1): 0, (1, 0): 1, (1, 1): 6}
                        for bk in blocks:
                            kpar = par if bk == 1 else 1 - par
                            for h in range(2):
                                bnk = sbank[(bk, h)]
                                pe(mm_group([
                                    (ps[bnk][:, :], kT[kpar][h * 64:(h + 1) * 64, :], qT[h * 64:(h + 1) * 64, :], True, False),
                                    (ps[bnk][:, :], ident_b[:], mb[:, bk, :], False, True)]),
                                   r=["kT%d" % kpar, "qT", "ident_b", "mb"], w=["ps%d" % bnk])
                                actop(_call("activation",
                                    out=PT[bk][h], in_=ps[bnk][:], func=AF.Exp, scale=HD ** -0.5),
                                    r=["ps%d" % bnk], w=["PT%d%d" % (bk, h)])
                        for h in range(2):
                            specs = []
                            for gg in range(4):
                                first = True
                                for bk in blocks:
                                    kpar = par if bk == 1 else 1 - par
                                    specs.append((ps[2 + h][:, gg * 65:(gg + 1) * 65],
                                                  PT[bk][h][:, gg * 128:(gg + 1) * 128], vaug[kpar][:, h, :],
                                                  first, bk == 1))
                                    first = False
                            pe(mm_group(specs), r=["PT%d%d" % (bk, h) for bk in blocks] + ["vaug0", "vaug1"],
                               w=["ps%d" % (2 + h)])
                            O3 = ps[2 + h][:, 0:260].rearrange("p (g d) -> p g d", d=65)
                            dve(_call("tensor_tensor",
                                out=smv(48 + h * 4, 4), in0=O3[:, :, 64], in1=esink[:, l * 8 + h * 4: l * 8 + h * 4 + 4],
                                op=ALU.add), r=["ps%d" % (2 + h), "esink"], w=["den%d" % h])
                            dve(_call("reciprocal", out=smv(56 + h * 4, 4), in_=smv(48 + h * 4, 4)),
                                r=["den%d" % h], w=["rden%d" % h])
                            dve(_call("tensor_tensor",
                                out=attn[:, h * 256:(h + 1) * 256].rearrange("p (g d) -> p g d", d=64),
                                in0=O3[:, :, 0:64], in1=smv(56 + h * 4, 4).unsqueeze(2).broadcast_to([128, 4, 64]),
                                op=ALU.mult), r=["ps%d" % (2 + h), "rden%d" % h], w=["attn%d" % h])
                        pe(tr_group([(ps[5][:, j * 128:(j + 1) * 128], attn[:, j * 128:(j + 1) * 128]) for j in range(4)]),
                           r=["attn0", "attn1", "ident_f"], w=["ps5"])
                        actop(_call("activation", out=mixT[:, 0:4, :], in_=ps[5][:].rearrange("p (k t) -> p k t", k=4),
                                                     func=AF.Identity), r=["ps5"], w=["mixTa"])
                        specs = []
                        for gg in range(4):
                            if i == 0:
                                specs.append((ps[4][:, gg * 128:(gg + 1) * 128], usb[par][:, gg * 128:(gg + 1) * 128],
                                              bt[:, 8 + gg, :], True, True))
                            else:
                                specs.append((ps[4][:, gg * 128:(gg + 1) * 128], usb[par][:, gg * 128:(gg + 1) * 128],
                                              bt[:, gg, :], True, False))
                                specs.append((ps[4][:, gg * 128:(gg + 1) * 128], usb[1 - par][:, gg * 128:(gg + 1) * 128],
                                              bt[:, 4 + gg, :], False, True))
                        pe(mm_group(specs), r=["usb0", "usb1", "bt"], w=["ps4"])
                        dve(_call("tensor_copy", out=pooledT, in_=ps[4][:]), r=["ps4"], w=["pooledT"])
                        pe(mm_group([(ps[7][:, gg * 128:(gg + 1) * 128], wpool[:, l * 4 + gg, :],
                                      pooledT[:, gg * 128:(gg + 1) * 128], True, True) for gg in range(4)]),
                           r=["pooledT", "wpool"], w=["ps7"])
                        for gg in range(4):
                            dve(_call("tensor_scalar",
                                out=mixT[:, 4 + gg, :], in0=ps[7][:, gg * 128:(gg + 1) * 128],
                                scalar1=pb_col[:, l * 4 + gg: l * 4 + gg + 1], scalar2=psc_col[:, l * 4 + gg: l * 4 + gg + 1],
                                op0=ALU.add, op1=ALU.mult), r=["ps7", "pb_col", "psc_col"], w=["mixTb%d" % gg])
                        for n in range(2):
                            pe(mm_group([(ps[n][:, :], mixT[:, k, :], w_out[:, k, n * 512:(n + 1) * 512], k == 0, k == 7)
                                         for k in range(8)]),
                               r=["mixTa", "mixTb0", "mixTb1", "mixTb2", "mixTb3", "w_out0", "w_out512"], w=["ps%d" % n])
                            dve(_call("tensor_tensor", out=x1[:, n * 512:(n + 1) * 512], in0=ps[n][:],
                                                               in1=G1bc[:, n * 512:(n + 1) * 512], op=ALU.mult),
                                r=["ps%d" % n, "G1bc"], w=["x1_%d" % n])
                            dve(_call("tensor_tensor", out=x1[:, n * 512:(n + 1) * 512],
                                                               in0=x1[:, n * 512:(n + 1) * 512],
                                                               in1=xt[:, n * 512:(n + 1) * 512], op=ALU.add),
                                r=["x1_%d" % n, xtk], w=["x1_%d" % n])
                        dma_sp("x1st", x1_d[r0:r0 + 128, :], x1, r=["x1_0", "x1_1"])
                        actop(_call("activation", out=junk, in_=x1, func=AF.Square, accum_out=smv(4)),
                              r=["x1_0", "x1_1"], w=["junk", "ss2"])
                        actop(_call("activation", out=smv(5), in_=smv(4), func=AF.Sqrt, scale=1.0 / D,
                                                     bias=cst[:, 0:1]), r=["ss2", "cst0"], w=["sd2"])
                        dve(_call("reciprocal", out=smv(6), in_=smv(5)), r=["sd2"], w=["rstd2"])
                        dve(_call("tensor_scalar", out=xn2, in0=x1, scalar1=smv(6), scalar2=None, op0=ALU.mult),
                            r=["x1_0", "x1_1", "rstd2"], w=["xn2"])
                        for b in range(2):
                            bnk = 5 + b
                            pe(tr_group([(ps[bnk][:, j * 128:(j + 1) * 128], xn2[:, (b * 4 + j) * 128:(b * 4 + j + 1) * 128])
                                         for j in range(4)]), r=["xn2", "ident_f"], w=["ps%d" % bnk])
                            for j in range(4):
                                k = b * 4 + j
                                if j % 2 == 1:
                                    dve(_call("tensor_scalar",
                                        out=h2Tf[:, k, :], in0=ps[bnk][:, j * 128:(j + 1) * 128],
                                        scalar1=A2[:, l * 8 + k: l * 8 + k + 1], scalar2=S2c(l, k),
                                        op0=ALU.mult, op1=ALU.add), r=["ps%d" % bnk, "A2", "modT"], w=["h2Tf%d" % k])
                                else:
                                    actop(_call("activation",
                                        out=h2Tf[:, k, :], in_=ps[bnk][:, j * 128:(j + 1) * 128], func=AF.Identity,
                                        scale=A2[:, l * 8 + k: l * 8 + k + 1], bias=S2c(l, k)),
                                        r=["ps%d" % bnk, "A2", "modT"], w=["h2Tf%d" % k])

                        dve(_call("tensor_tensor", out=xn, in0=xn2, in1=A2bc[:], op=ALU.mult),
                            r=["xn2", "A2bc", "xn"], w=["xn"])
                        dve(_call("tensor_tensor", out=h2row, in0=xn, in1=S2bc[:], op=ALU.add),
                            r=["xn", "S2bc"], w=["h2row"])
                        dma_sp("h2st", h2buf_d[r0:r0 + 128, :], h2row, r=["h2row"])
                        pe(mm_group([(ps[7][:, 0:NE], h2Tf[:, k, :], rw[:, l * 8 + k, :], k == 0, k == 7) for k in range(8)]),
                           r=["h2Tf%d" % k for k in range(8)] + ["rw"], w=["ps7"])
                        dve(_call("tensor_tensor", out=lg, in0=ps[7][:, 0:NE], in1=rb_bc[:, l * NE:(l + 1) * NE],
                                  op=ALU.add), r=["ps7", "rb_bc"], w=["lg"])
                        dve(_call("max", out=smv(64, 8), in_=lg), r=["lg"], w=["top8"])
                        dve(_call("tensor_scalar", out=smv(72), in0=smv(64), scalar1=-1.0, scalar2=None, op0=ALU.mult),
                            r=["top8"], w=["nmax"])
                        dve(_call("tensor_scalar", out=msk, in0=lg, scalar1=smv(67), scalar2=None, op0=ALU.is_ge),
                            r=["lg", "top8"], w=["msk"])
                        actop(_call("activation", out=smv(80, 4), in_=smv(64, 4), func=AF.Exp, bias=smv(72)),
                              r=["top8", "nmax"], w=["e4x"])
                        dve(_call("tensor_reduce", out=smv(84), in_=smv(80, 4), axis=AX.X, op=ALU.add), r=["e4x"], w=["gden"])
                        dve(_call("reciprocal", out=smv(85), in_=smv(84)), r=["gden"], w=["grd"])
                        dve(_call("tensor_scalar", out=G4[:, i, :], in0=smv(80, 4), scalar1=smv(85), scalar2=None,
                                  op0=ALU.mult), r=["e4x", "grd"], w=["G4"])
                        pe(mm_group([(ps[7][:, 64:96], ustrict[:], msk, True, True),
                                     (ps[7][:, 96:128], ones_f[:], msk, True, True)]),
                           r=["msk", "ustrict", "ones_f"], w=["ps7"])
                        dve(_call("tensor_tensor", out=exv, in0=ps[7][:, 64:96], in1=cum[:], op=ALU.add),
                            r=["ps7", "cum"], w=["rankt"])
                        dve(_call("tensor_tensor", out=cum[:], in0=cum[:], in1=ps[7][:, 96:128], op=ALU.add),
                            r=["ps7", "cum"], w=["cum"])
                        lgb = lg.unsqueeze(1).broadcast_to([128, 4, NE])
                        t8b = smv(64, 4).unsqueeze(2).broadcast_to([128, 4, NE])
                        dve(_call("tensor_tensor", out=sel4, in0=lgb, in1=t8b, op=ALU.is_equal), r=["lg", "top8"], w=["sel4"])
                        dve(_call("tensor_tensor", out=prod4, in0=sel4, in1=exv.unsqueeze(1).broadcast_to([128, 4, NE]),
                                  op=ALU.mult), r=["sel4", "rankt"], w=["prod4"])
                        dve(_call("tensor_reduce", out=rank4[:, i, :], in_=prod4, axis=AX.X, op=ALU.add), r=["prod4"], w=["rank4"])
                        dve(_call("tensor_tensor", out=prod4, in0=sel4, in1=iotaE[:].unsqueeze(1).broadcast_to([128, 4, NE]),
                                  op=ALU.mult), r=["sel4", "iotaE"], w=["prod4"])
                        dve(_call("tensor_reduce", out=e4[:, i, :], in_=prod4, axis=AX.X, op=ALU.add), r=["prod4"], w=["e4"])
                        if i == 0:
                            chk("tile0")
                S.phase_barrier()
                chk("mixer")

                cmp3 = big[:, 0:NE * 16].rearrange("p (e m) -> p e m", m=16)
                dve(_call("tensor_tensor", out=cmp3, in0=cum[:].unsqueeze(2).broadcast_to([128, NE, 16]),
                          in1=thr512[:].unsqueeze(1).broadcast_to([128, NE, 16]), op=ALU.is_gt),
                    r=["cum", "thr512"], w=["big"])
                dve(_call("tensor_reduce", out=rt_pad[:], in_=cmp3, axis=AX.X, op=ALU.add), r=["big"], w=["rt_pad"])
                dve(_call("tensor_scalar", out=rt_pad[:], in0=rt_pad[:], scalar1=float(BLK), scalar2=None, op0=ALU.mult),
                    r=["rt_pad"], w=["rt_pad"])
                dve(_call("tensor_tensor_scan", out=rt_pend[:], data0=ones_f[:, 0:NE], data1=rt_pad[:], initial=0.0,
                          op0=ALU.mult, op1=ALU.add), r=["rt_pad", "ones_f"], w=["rt_pend"])
                dve(_call("tensor_tensor", out=rt_pst[:], in0=rt_pend[:], in1=rt_pad[:], op=ALU.subtract),
                    r=["rt_pend", "rt_pad"], w=["rt_pst"])
                e4f = e4[:].rearrange("p t k -> p (t k)")
                destf = rank4[:].rearrange("p t k -> p (t k)")
                dtmp = big[:, 0:nt * 4]
                for ee in range(NE):
                    dve(_call("tensor_scalar", out=dtmp, in0=e4f, scalar1=float(ee), scalar2=rt_pst[:, ee:ee + 1],
                              op0=ALU.is_equal, op1=ALU.mult), r=["e4", "rt_pst"], w=["big"])
                    dve(_call("tensor_tensor", out=destf, in0=destf, in1=dtmp, op=ALU.add), r=["big", "rank4"], w=["rank4"])
                dve(_call("tensor_copy", out=dest_i[:].rearrange("p t k -> p (t k)"), in_=destf), r=["rank4"], w=["dest_i"])
                cmpb = big[:, 0:NB * NE].rearrange("p (b e) -> p b e", e=NE)
                dve(_call("tensor_tensor", out=cmpb, in0=bvals[:].unsqueeze(2).broadcast_to([128, NB, NE]),
                          in1=rt_pend[:].unsqueeze(1).broadcast_to([128, NB, NE]), op=ALU.is_ge),
                    r=["bvals", "rt_pend"], w=["big"])
                dve(_call("tensor_reduce", out=ebf[:], in_=cmpb, axis=AX.X, op=ALU.add), r=["big"], w=["ebf"])
                dve(_call("tensor_scalar", out=ebf[:], in0=ebf[:], scalar1=float(NE - 1), scalar2=float(l * NE),
                          op0=ALU.min, op1=ALU.add), r=["ebf"], w=["ebf"])
                dve(_call("tensor_copy", out=Id_i[:], in_=ebf[:]), r=["ebf"], w=["Id_i"])
                dve(_call("tensor_scalar", out=big[:, 0:NB], in0=ebf[:], scalar1=128.0, scalar2=pidx[:, 0:1],
                          op0=ALU.mult, op1=ALU.add), r=["ebf", "pidx"], w=["big"])
                dve(_call("tensor_copy", out=Ib_i[:], in_=big[:, 0:NB]), r=["big"], w=["Ib_i"])
                iw3 = big[:, 0:NB * 8].rearrange("p (b k) -> p b k", k=8)
                dve(_call("tensor_scalar", out=ebf[:], in0=ebf[:], scalar1=1024.0, scalar2=None, op0=ALU.mult),
                    r=["ebf", "Ib_i", "big"], w=["ebf"])
                dve(_call("tensor_tensor", out=iw3, in0=ebf[:].unsqueeze(2).broadcast_to([128, NB, 8]),
                          in1=pidx[:].unsqueeze(1).broadcast_to([128, NB, 8]), op=ALU.add), r=["ebf", "pidx"], w=["big"])
                dve(_call("tensor_copy", out=IW_i[:], in_=iw3), r=["big"], w=["IW_i"])
                dve(_call("memset", cum[:], 0.0), r=["rt_pad"], w=["cum"])
                S.phase_barrier()
                chk("route")

                for i in range(nt):
                    q = i % 2
                    dma_sp("hs%d" % q, hs[q], h2buf_d[i * 128:(i + 1) * 128, :], w=["hs%d" % q])
                    for k4 in range(4):
                        S.add("pool", _call("indirect_dma_start", out=hslots_d,
                                            out_offset=bass.IndirectOffsetOnAxis(ap=dest_i[:, i, k4:k4 + 1], axis=0),
                                            in_=hs[q], in_offset=None), ["hs%d" % q, "dest_i"], [], dma="sc%d" % q)
                S.phase_barrier()
                chk("scatter")

                ycount = 0
                gcount = 0
                def load_rows(bb):
                    hq = bb % 2
                    dma_sp("hr%d" % hq, hrows[hq], hslots_d[bb * BLK:(bb + 1) * BLK, :].rearrange("(t p) d -> p t d", p=128),
                           w=["hrows%d" % hq])

                def emit_transposes(bb):
                    hq = bb % 2
                    for k in range(8):
                        tb = 6 + k % 2
                        psb = ps[tb][:].bitcast(BF16)
                        pe(tr_group_b([(psb[:, tt * 128:(tt + 1) * 128], hrows[hq][:, tt, k * 128:(k + 1) * 128])
                                       for tt in range(4)]), r=["hrows%d" % hq, "ident_b"], w=["ps%d" % tb])
                        if k % 2 == 0:
                            dve(_call("tensor_copy", out=h2Tb[hq][:, k, :], in_=psb[:, 0:512]), r=["ps%d" % tb],
                                w=["h2Tb%d_%d" % (hq, k)])
                        else:
                            actop(_call("activation", out=h2Tb[hq][:, k, :], in_=psb[:, 0:512], func=AF.Identity),
                                  r=["ps%d" % tb], w=["h2Tb%d_%d" % (hq, k)])

                load_rows(0)
                for b in range(NB):
                    hb = b % 2
                    if b + 1 < NB:
                        load_rows(b + 1)
                    for k in range(8):
                        S.add("pool", _call("indirect_dma_start", out=Wgu[hb][:, k, :], out_offset=None, in_=wgu2d,
                                            in_offset=bass.IndirectOffsetOnAxis(ap=IW_i[:, b, k:k + 1], axis=0)),
                              ["IW_i"], ["Wgu%d_%d" % (hb, k)], dma="wgu%d" % hb)
                    S.add("pool", _call("indirect_dma_start", out=bgub[hb][:], out_offset=None, in_=bgu_d,
                                        in_offset=bass.IndirectOffsetOnAxis(ap=Ib_i[:, b:b + 1], axis=0)),
                          ["Ib_i"], ["bgub%d" % hb], dma="bg%d" % hb)
                    S.add("pool", _call("indirect_dma_start", out=bdbc[hb][:], out_offset=None, in_=bdn_d,
                                        in_offset=bass.IndirectOffsetOnAxis(ap=Id_i[:, b:b + 1], axis=0)),
                          ["Id_i"], ["bdbc%d" % hb], dma="bd%d" % hb)
                    for j in range(8):
                        S.add("pool", _call("indirect_dma_start", out=Wd[hb][:, j, :], out_offset=None, in_=wdn2d,
                                            in_offset=bass.IndirectOffsetOnAxis(ap=IW_i[:, b, j:j + 1], axis=0)),
                              ["IW_i"], ["Wd%d_%d" % (hb, j)], dma="wd%d" % hb)
                    if b == 0:
                        emit_transposes(0)
                    dve(_call("tensor_scalar", out=bgs[hb][:], in0=bgub[hb][:, 0:8], scalar1=ALPHA, scalar2=None,
                              op0=ALU.mult), r=["bgub%d" % hb], w=["bgs%d" % hb])
                    dve(_call("tensor_scalar", out=bu1[hb][:], in0=bgub[hb][:, 8:16], scalar1=1.0, scalar2=None,
                              op0=ALU.add), r=["bgub%d" % hb], w=["bu1%d" % hb])
                    h2keys = ["h2Tb%d_%d" % (hb, k) for k in range(8)]
                    wgkeys = ["Wgu%d_%d" % (hb, k) for k in range(8)]
                    wdkeys = ["Wd%d_%d" % (hb, j) for j in range(8)]
                    for fj in range(8):
                        ab = act_sb[fj // 4]
                        j = fj % 4
                        gb = gcount % 2
                        gcount += 1
                        pe(mm_group([(ps[gb][:, :], Wgu[hb][:, k, fj * 128:(fj + 1) * 128], h2Tb[hb][:, k, :], k == 0, k == 7)
                                     for k in range(8)]), r=wgkeys + h2keys, w=["ps%d" % gb])
                        pe(mm_group([(ps[2 + gb][:, :], Wgu[hb][:, k, D + fj * 128: D + (fj + 1) * 128], h2Tb[hb][:, k, :],
                                      k == 0, k == 7) for k in range(8)]), r=wgkeys + h2keys, w=["ps%d" % (2 + gb)])
                        actop(_call("activation", out=sg_t[:], in_=ps[gb][:], func=AF.Sigmoid, scale=ALPHA,
                                    bias=bgs[hb][:, fj:fj + 1]), r=["ps%d" % gb, "bgs%d" % hb], w=["sg"])
                        dve(_call("tensor_scalar", out=gl_t[:], in0=ps[gb][:], scalar1=bgub[hb][:, fj:fj + 1], scalar2=LIMIT,
                                  op0=ALU.add, op1=ALU.min), r=["ps%d" % gb, "bgub%d" % hb], w=["gl"])
                        dve(_call("tensor_scalar", out=t1_t[:], in0=ps[2 + gb][:], scalar1=bu1[hb][:, fj:fj + 1],
                                  scalar2=1.0 - LIMIT, op0=ALU.add, op1=ALU.max), r=["ps%d" % (2 + gb), "bu1%d" % hb], w=["t1"])
                        dve(_call("scalar_tensor_tensor", out=aa_t[:], in0=sg_t[:], scalar=SIG_MAX, in1=gl_t[:],
                                  op0=ALU.min, op1=ALU.mult), r=["sg", "gl"], w=["aa"])
                        dve(_call("scalar_tensor_tensor", out=ab[:, j, :], in0=t1_t[:], scalar=1.0 + LIMIT, in1=aa_t[:],
                                  op0=ALU.min, op1=ALU.mult), r=["t1", "aa"], w=["act%d" % fj])
                    if b + 1 < NB:
                        emit_transposes(b + 1)
                    for tt in range(4):
                        for dh in range(2):
                            yb = 4 + ycount % 2
                            ycount += 1
                            specs = []
                            for fj in range(8):
                                specs.append((ps[yb][:, :], act_sb[fj // 4][:, fj % 4, tt * 128:(tt + 1) * 128],
                                              Wd[hb][:, fj, dh * 512:(dh + 1) * 512], fj == 0, fj == 7))
                            pe(mm_group(specs), r=["act%d" % fj for fj in range(8)] + wdkeys, w=["ps%d" % yb])
                            dve(_call("tensor_tensor", out=ystage[:, tt, dh * 512:(dh + 1) * 512], in0=ps[yb][:],
                                      in1=bdbc[hb][:, dh * 512:(dh + 1) * 512], op=ALU.add),
                                r=["ps%d" % yb, "bdbc%d" % hb], w=["ystage%d_%d" % (tt, dh)])
                    dma_sp("yst", yslots_d[b * BLK:(b + 1) * BLK, :].rearrange("(t p) d -> p t d", p=128), ystage,
                           r=["ystage%d_%d" % (tt, dh) for tt in range(4) for dh in range(2)])
                    if b == 0:
                        chk("blk0")
                S.phase_barrier()
                chk("blocks")

                WRf = WR[:, 0:16384].bitcast(F32)
                ykb = [yk, [WRf[:, k * 1024:(k + 1) * 1024] for k in range(4)]]
                ex1b = [ex1, WRf[:, 4096:5120]]
                eab = [ea, WRf[:, 5120:6144]]

                def comb_loads(ii):
                    q = ii % 2
                    dma_sp("ex1_%d" % q, ex1b[q], x1_d[ii * 128:(ii + 1) * 128, :], w=["ex1_%d" % q])
                    for k4 in range(4):
                        S.add("pool", _call("indirect_dma_start", out=ykb[q][k4], out_offset=None, in_=yslots_d,
                                            in_offset=bass.IndirectOffsetOnAxis(ap=dest_i[:, ii, k4:k4 + 1], axis=0)),
                              ["dest_i"], ["yk%d_%d" % (q, k4)], dma="yk%d_%d" % (q, k4))

                comb_loads(0)
                for i in range(nt):
                    r0 = i * 128
                    q = i % 2
                    if i + 1 < nt:
                        comb_loads(i + 1)
                    ykq, eaq, ex1q = ykb[q], eab[q], ex1b[q]
                    actop(_call("activation", out=eaq, in_=ykq[0], func=AF.Identity, scale=G4[:, i, 0:1]),
                          r=["yk%d_0" % q, "G4"], w=["ea%d" % q])
                    for k4 in range(1, 4):
                        dve(_call("scalar_tensor_tensor", out=eaq, in0=ykq[k4], scalar=G4[:, i, k4:k4 + 1], in1=eaq,
                                  op0=ALU.mult, op1=ALU.add), r=["yk%d_%d" % (q, k4), "G4", "ea%d" % q], w=["ea%d" % q])
                    S.add("pool", _call("tensor_tensor", out=eaq, in0=eaq, in1=G2bc[:], op=ALU.mult),
                          ["ea%d" % q, "G2bc"], ["ea%d" % q])
                    S.add("pool", _call("tensor_tensor", out=eaq, in0=eaq, in1=ex1q, op=ALU.add),
                          ["ea%d" % q, "ex1_%d" % q], ["ea%d" % q])
                    dma_sp("ost%d" % q, dst_d[r0:r0 + 128, :], eaq, r=["ea%d" % q])
                S.phase_barrier()
                chk("layer")
        except _Stop:
            S.phase_barrier()


        S.add("sp", lambda e: _Nop())
        S.emit(nc, st)
    return nc


def _col(v):
    v = np.asarray(v, np.float32)
    lead = int(np.prod(v.shape[:-1])) if v.ndim > 1 else 1
    n = v.shape[-1] // 128
    return np.ascontiguousarray(v.reshape(lead * n, 128).T)


def make_in_maps(inputs, ncores, ntok, depth):
    ident, mbc, btc = _const_tables()
    us_, io_, th_, bv_, pi_ = _route_tables(ntok)
    f = lambda k: np.ascontiguousarray(np.asarray(inputs[k], np.float32)[:depth])
    shared = {
        "ada_w": f("ada_w"),
        "adab_col": _col(f("ada_b")),
        "n1g_col": _col(f("norm1_g")),
        "n2g_col": _col(f("norm2_g")),
        "w_in": f("w_in"),
        "gqk": np.ascontiguousarray(np.concatenate([f("q_norm_g"), f("k_norm_g")], axis=1).reshape(1, depth * 128)),
        "sinks": np.ascontiguousarray(f("attn_sinks").reshape(1, depth * 8)),
        "pool_w": f("pool_w"),
        "pb_col": _col(f("pool_b")),
        "psc_col": _col(f("pool_scale")),
        "w_out": f("w_out"),
        "router_w": f("router_w"),
        "router_b": np.ascontiguousarray(f("router_b").reshape(1, depth * NE)),
        "w_gu": f("expert_w_gu"),
        "bgu_t": np.ascontiguousarray(f("expert_b_gu").reshape(depth, NE, 16, 128).transpose(0, 1, 3, 2).reshape(depth * NE * 128, 16)),
        "w_down": f("expert_w_down"),
        "bdn_t": np.ascontiguousarray(f("expert_b_down").reshape(depth * NE, D)),
        "cst_ident": ident,
        "cst_mb": np.ascontiguousarray(mbc.reshape(128, 1024)),
        "cst_bt": btc,
        "cst_ustrict": us_, "cst_iota": io_, "cst_thr": th_, "cst_bvals": bv_, "cst_pidx": pi_,
    }
    x = np.asarray(inputs["x"], np.float32)
    c = np.asarray(inputs["c"], np.float32)
    pos = np.asarray(inputs["positions"], np.int32)
    maps = []
    for b in range(ncores):
        m = dict(shared)
        m["x"] = np.ascontiguousarray(x[b, :ntok])
        m["ccol"] = np.ascontiguousarray(c[b].reshape(8, 128).T)
        m["poscol"] = np.ascontiguousarray(pos[b, :ntok].reshape(ntok // 128, 128).T)
        maps.append(m)
    return maps


_NC_CACHE = {}


def run(inputs, ncores, ntok, depth, trace=False, stop=None):
    key = (ntok, depth, stop)
    if key not in _NC_CACHE:
        _NC_CACHE[key] = build_program(ntok, depth, stop)
    nc = _NC_CACHE[key]
    maps = make_in_maps(inputs, ncores, ntok, depth)
    res = run_bass_kernel_spmd(nc, maps, core_ids=list(range(ncores)), trace=trace)
    out = np.stack([np.asarray(r["out"]) for r in res.results], axis=0)
    return out.astype(np.float32), res


def kernel(**inputs):
    out, _ = run(inputs, 8, 8192, 2)
    return out
```
